# Optimizing a Trainium2 kernel written in Bass

```python
import jax
import jax.numpy as jnp
from jax import lax
import numpy as np

D_MODEL = 2048
BATCH = 8
SEQ = 4096
DEPTH = 1

HEAD_DIM = 128
M_WIDTH = D_MODEL // 4
A_WIDTH = (D_MODEL - M_WIDTH) // 2
B_WIDTH = D_MODEL - M_WIDTH - A_WIDTH
M_HEADS = M_WIDTH // HEAD_DIM
A_GROUPS = A_WIDTH // HEAD_DIM
B_HEADS = B_WIDTH // HEAD_DIM
D_PROJ = 2 * A_WIDTH + 3 * B_WIDTH + M_WIDTH
CHUNK = 128
DILATED_PATTERNS = ((128, 1), (512, 4), (2048, 16))
ATT_BLOCK = 128
N_MEM = 256
N_EXPERTS = 32
TOP_K = 4
D_FF = D_MODEL
SWIGLU_LIMIT = 7.0
SWIGLU_ALPHA = 1.702
MOE_BLOCK = 512
LN_EPS = 1e-5
NEG_INF = -1e30
DN_ALPHA = (2.0 * DEPTH) ** 0.25
DN_BETA = (8.0 * DEPTH) ** -0.25

kernel_name = "hymba_gmlp_longnet_mem_moe_deepnorm"


def layer_norm(x, g, b):
    xf = x.astype(jnp.float32)
    mu = jnp.mean(xf, axis=-1, keepdims=True)
    var = jnp.mean(jnp.square(xf - mu), axis=-1, keepdims=True)
    return ((xf - mu) * lax.rsqrt(var + LN_EPS)).astype(x.dtype) * g + b


def rms_norm(x, g):
    xf = x.astype(jnp.float32)
    return (xf * lax.rsqrt(jnp.mean(jnp.square(xf), axis=-1, keepdims=True) + LN_EPS)).astype(x.dtype) * g


def chunked_spatial_gating(a, w_spatial, b_spatial, ln_g, ln_b):
    bsz, seq, _ = a.shape
    u, v = jnp.split(a, 2, axis=-1)
    v = layer_norm(v, ln_g, ln_b)
    v = v.reshape(bsz, seq // CHUNK, CHUNK, A_GROUPS, HEAD_DIM)
    causal = jnp.tril(jnp.ones((CHUNK, CHUNK), dtype=bool))
    w = jnp.where(causal[None], w_spatial, 0)
    mixed = jnp.einsum('gts,bnsgc->bntgc', w, v) + b_spatial.T[None, None, :, :, None]
    return u * mixed.reshape(bsz, seq, A_WIDTH)


def dilated_window_attention(q, k, v, window, dilation):
    bsz, seq, nh, dh = q.shape
    span = window // dilation
    sub_len = seq // dilation
    nb = -(-sub_len // ATT_BLOCK)
    lp = nb * ATT_BLOCK

    def to_sub(t):
        t = t.reshape(bsz, sub_len, dilation, nh, dh).transpose(0, 2, 1, 3, 4)
        return jnp.pad(t, ((0, 0), (0, 0), (0, lp - sub_len), (0, 0), (0, 0)))

    def windowed(t):
        prev = jnp.pad(t, ((0, 0), (0, 0), (ATT_BLOCK, 0), (0, 0), (0, 0)))[:, :, :lp]
        prev = prev.reshape(bsz, dilation, nb, ATT_BLOCK, nh, dh)
        cur = t.reshape(bsz, dilation, nb, ATT_BLOCK, nh, dh)
        return jnp.concatenate([prev, cur], axis=3)

    qb = to_sub(q).reshape(bsz, dilation, nb, ATT_BLOCK, nh, dh)
    kw = windowed(to_sub(k))
    vw = windowed(to_sub(v))
    s = jnp.einsum('brnqhc,brnkhc->brnhqk', qb, kw,
                   preferred_element_type=jnp.float32) * (HEAD_DIM ** -0.5)
    qi = jnp.arange(ATT_BLOCK)[:, None]
    kj = jnp.arange(2 * ATT_BLOCK)[None, :]
    dist = qi + ATT_BLOCK - kj
    key_pos = jnp.arange(nb)[:, None, None] * ATT_BLOCK - ATT_BLOCK + kj[None]
    mask = (dist >= 0) & (dist <= span) & (key_pos >= 0)
    s = jnp.where(mask[None, None, :, None], s, NEG_INF)
    lse = jax.nn.logsumexp(s, axis=-1)
    p = jnp.exp(s - lse[..., None])
    o = jnp.einsum('brnhqk,brnkhc->brnqhc', p.astype(v.dtype), vw)

    def from_sub(t):
        t = jnp.moveaxis(t[:, :, :sub_len], 1, 2)
        return t.reshape(bsz, seq, *t.shape[3:])

    o = from_sub(o.reshape(bsz, dilation, lp, nh, dh))
    lse = from_sub(lse.transpose(0, 1, 2, 4, 3).reshape(bsz, dilation, lp, nh))
    return o, lse


def dilated_mixture_attention(q, k, v):
    outs, lses = [], []
    for window, dilation in DILATED_PATTERNS:
        o, lse = dilated_window_attention(q, k, v, window, dilation)
        outs.append(o)
        lses.append(lse)
    wts = jax.nn.softmax(jnp.stack(lses, axis=0), axis=0)
    o = jnp.einsum('pbsh,pbshc->bshc', wts, jnp.stack(outs, axis=0).astype(jnp.float32))
    return o.astype(q.dtype)


def memory_cross_attention(q, mem, w_mem_kv):
    bsz, n_mem, _ = mem.shape
    kv = (mem @ w_mem_kv).reshape(bsz, n_mem, 2, M_HEADS, HEAD_DIM)
    k, v = kv[:, :, 0], kv[:, :, 1]
    s = jnp.einsum('bshc,bmhc->bhsm', q, k, preferred_element_type=jnp.float32) * (HEAD_DIM ** -0.5)
    p = jax.nn.softmax(s, axis=-1)
    return jnp.einsum('bhsm,bmhc->bshc', p.astype(v.dtype), v)


def mixing_sublayer(x, mem, w_in, w_spatial, b_spatial, a_ln_g, a_ln_b, w_mem_kv,
                    norm_a_g, norm_b_g, norm_m_g, w_out):
    bsz, seq, _ = x.shape
    z = x @ w_in
    o1 = 2 * A_WIDTH
    za, zq, zk, zv, zm = jnp.split(z, [o1, o1 + B_WIDTH, o1 + 2 * B_WIDTH, o1 + 3 * B_WIDTH], axis=-1)
    y_a = chunked_spatial_gating(jax.nn.gelu(za), w_spatial, b_spatial, a_ln_g, a_ln_b)
    heads = lambda t, h: t.reshape(bsz, seq, h, HEAD_DIM)
    y_b = dilated_mixture_attention(heads(zq, B_HEADS), heads(zk, B_HEADS), heads(zv, B_HEADS))
    y_m = memory_cross_attention(heads(zm, M_HEADS), mem, w_mem_kv)
    y = jnp.concatenate([rms_norm(y_a, norm_a_g),
                         rms_norm(y_b.reshape(bsz, seq, B_WIDTH), norm_b_g),
                         rms_norm(y_m.reshape(bsz, seq, M_WIDTH), norm_m_g)], axis=-1)
    return y @ w_out


def moe_ffn(h, w_router, b_router, w_gate_up, b_gate_up, w_down, b_down):
    bsz, seq, d = h.shape
    n_tok = bsz * seq
    hf = h.reshape(n_tok, d)
    logits = (hf @ w_router + b_router).astype(jnp.float32)
    top_val, top_idx = lax.top_k(logits, TOP_K)
    gates = jax.nn.softmax(top_val, axis=-1).astype(h.dtype)
    n_assign = n_tok * TOP_K
    flat_e = top_idx.reshape(-1)
    flat_tok = jnp.arange(n_assign, dtype=jnp.int32) // TOP_K
    flat_g = gates.reshape(-1)
    order = jnp.argsort(flat_e)
    sorted_e = flat_e[order]
    counts = jnp.bincount(flat_e, length=N_EXPERTS)
    padded = (counts + MOE_BLOCK - 1) // MOE_BLOCK * MOE_BLOCK
    start = jnp.cumsum(counts) - counts
    pad_end = jnp.cumsum(padded)
    pad_start = pad_end - padded
    rank = jnp.arange(n_assign, dtype=jnp.int32) - start[sorted_e]
    dest = pad_start[sorted_e] + rank
    n_blocks = (n_assign + N_EXPERTS * (MOE_BLOCK - 1)) // MOE_BLOCK + 1
    n_rows = n_blocks * MOE_BLOCK
    row_tok = jnp.zeros((n_rows,), jnp.int32).at[dest].set(flat_tok[order])
    row_gate = jnp.zeros((n_rows,), h.dtype).at[dest].set(flat_g[order])
    block_expert = jnp.minimum(
        jnp.searchsorted(pad_end, jnp.arange(n_blocks, dtype=jnp.int32) * MOE_BLOCK, side='right'),
        N_EXPERTS - 1)
    xs = hf[row_tok].reshape(n_blocks, MOE_BLOCK, d)

    def expert_block(args):
        xb, e = args
        gu = xb @ w_gate_up[e] + b_gate_up[e]
        gate, up = jnp.split(gu, 2, axis=-1)
        gate = jnp.minimum(gate, SWIGLU_LIMIT)
        up = jnp.clip(up, -SWIGLU_LIMIT, SWIGLU_LIMIT)
        act = (up + 1.0) * (gate * jax.nn.sigmoid(SWIGLU_ALPHA * gate))
        return act @ w_down[e] + b_down[e]

    ys = lax.map(expert_block, (xs, block_expert)).reshape(n_rows, d) * row_gate[:, None]
    out = jax.ops.segment_sum(ys, row_tok, num_segments=n_tok)
    return out.reshape(bsz, seq, d)


def setup_inputs(seed: int = 0) -> dict:
    key = jax.random.key(seed)
    ks = jax.random.split(key, 22)
    nrm = lambda k, shape, scale: jax.random.normal(k, shape, jnp.float32) * scale
    L = DEPTH
    return {
        "x": nrm(ks[0], (BATCH, SEQ, D_MODEL), 1.0),
        "mem": nrm(ks[1], (BATCH, N_MEM, D_MODEL), 1.0),
        "w_in": nrm(ks[2], (L, D_MODEL, D_PROJ), D_MODEL ** -0.5),
        "w_spatial": nrm(ks[3], (L, A_GROUPS, CHUNK, CHUNK), CHUNK ** -0.5),
        "b_spatial": 1.0 + nrm(ks[4], (L, A_GROUPS, CHUNK), 0.1),
        "a_ln_g": 1.0 + nrm(ks[5], (L, A_WIDTH), 0.02),
        "a_ln_b": nrm(ks[6], (L, A_WIDTH), 0.02),
        "w_mem_kv": nrm(ks[7], (L, D_MODEL, 2 * M_WIDTH), D_MODEL ** -0.5),
        "norm_a_g": 1.0 + nrm(ks[8], (L, A_WIDTH), 0.02),
        "norm_b_g": 1.0 + nrm(ks[9], (L, B_WIDTH), 0.02),
        "norm_m_g": 1.0 + nrm(ks[10], (L, M_WIDTH), 0.02),
        "w_out": nrm(ks[11], (L, D_MODEL, D_MODEL), D_MODEL ** -0.5 * DN_BETA),
        "ln1_g": 1.0 + nrm(ks[12], (L, D_MODEL), 0.02),
        "ln1_b": nrm(ks[13], (L, D_MODEL), 0.02),
        "w_router": nrm(ks[14], (L, D_MODEL, N_EXPERTS), D_MODEL ** -0.5),
        "b_router": nrm(ks[15], (L, N_EXPERTS), 0.01),
        "w_gate_up": nrm(ks[16], (L, N_EXPERTS, D_MODEL, 2 * D_FF), D_MODEL ** -0.5),
        "b_gate_up": nrm(ks[17], (L, N_EXPERTS, 2 * D_FF), 0.01),
        "w_down": nrm(ks[18], (L, N_EXPERTS, D_FF, D_MODEL), D_FF ** -0.5 * DN_BETA),
        "b_down": nrm(ks[19], (L, N_EXPERTS, D_MODEL), 0.01),
        "ln2_g": 1.0 + nrm(ks[20], (L, D_MODEL), 0.02),
        "ln2_b": nrm(ks[21], (L, D_MODEL), 0.02),
    }


def reference(x, mem, w_in, w_spatial, b_spatial, a_ln_g, a_ln_b, w_mem_kv,
              norm_a_g, norm_b_g, norm_m_g, w_out, ln1_g, ln1_b,
              w_router, b_router, w_gate_up, b_gate_up, w_down, b_down, ln2_g, ln2_b):
    for l in range(DEPTH):
        mix = mixing_sublayer(x, mem, w_in[l], w_spatial[l], b_spatial[l], a_ln_g[l], a_ln_b[l],
                              w_mem_kv[l], norm_a_g[l], norm_b_g[l], norm_m_g[l], w_out[l])
        x = layer_norm(DN_ALPHA * x + mix, ln1_g[l], ln1_b[l])
        ffn = moe_ffn(x, w_router[l], b_router[l], w_gate_up[l], b_gate_up[l], w_down[l], b_down[l])
        x = layer_norm(DN_ALPHA * x + ffn, ln2_g[l], ln2_b[l])
    return x
```

```python
import numpy as np
import concourse.bass as bass
import concourse.mybir as mybir
from concourse.bass_utils import run_bass_kernel_spmd

F32 = mybir.dt.float32
BF16 = mybir.dt.bfloat16
I32 = mybir.dt.int32
AF = mybir.ActivationFunctionType
ALU = mybir.AluOpType
AX = mybir.AxisListType

ENGS = ["pe", "act", "dve", "pool", "sp"]
SEM_ROT = 12000
NDS = 6

S = 4096
D = 2048
NT = S // 128
GT = 512
NG = S // GT
NE = 32
NBLK = 64
DN_ALPHA = 2.0 ** 0.25
LN_EPS = 1e-5
SCALE = 128.0 ** -0.5


class Prog:
    def __init__(self, nc, same_engine_sync=True):
        self.nc = nc
        self.ops = {e: [] for e in ENGS}
        self.cnt = {e: 0 for e in ENGS}
        self.dcnt = {e: 0 for e in ENGS}
        self.lastw = {}
        self.readers = {}
        self.waited = {e: {} for e in ENGS}
        self.pend = {e: {} for e in ENGS}
        self.sems = {}
        self.same = same_engine_sync
        self.final = {}

    def _deps(self, eng, reads, writes, nosame):
        deps = set()
        for r in reads:
            t = self.lastw.get(r)
            if t is not None:
                deps.add(t)
        for w in writes:
            t = self.lastw.get(w)
            if t is not None:
                deps.add(t)
            for t in self.readers.get(w, ()):
                deps.add(t)
        out = dict(self.pend[eng])
        self.pend[eng] = {}
        for (s, v, e) in deps:
            if e == eng and (eng == "pe" or nosame or not self.same):
                continue
            out[s] = max(out.get(s, 0), v)
        res = []
        for s, v in out.items():
            if self.waited[eng].get(s, 0) >= v:
                continue
            self.waited[eng][s] = v
            res.append((s, v))
        return res

    def _commit(self, tok, reads, writes):
        for r in reads:
            self.readers.setdefault(r, []).append(tok)
        for w in writes:
            self.lastw[w] = tok
            self.readers[w] = []

    def op(self, eng, fn, reads=(), writes=(), nosame=False):
        waits = self._deps(eng, reads, writes, nosame)
        i = self.cnt[eng]
        self.cnt[eng] += 1
        s = "c_%s_%d" % (eng, i // SEM_ROT)
        tok = (s, i % SEM_ROT + 1, eng)
        self.ops[eng].append((waits, fn, s, 1))
        self._commit(tok, reads, writes)
        return tok

    def dma(self, eng, fn, reads=(), writes=()):
        waits = dict(self._deps(eng, reads, writes, False))
        j = self.dcnt[eng]
        self.dcnt[eng] += 1
        s = "d_%s_%d" % (eng, j % NDS)
        v = 16 * (j // NDS + 1)
        if v > 16 and self.waited[eng].get(s, 0) < v - 16:
            waits[s] = max(waits.get(s, 0), v - 16)
            self.waited[eng][s] = v - 16
        tok = (s, v, "dma_" + eng)
        self.ops[eng].append((list(waits.items()), fn, s, 16))
        self._commit(tok, reads, writes)
        self.final[s] = v
        return tok

    def _all_tokens(self):
        toks = []
        for e in ENGS:
            if self.cnt[e] > 0:
                i = self.cnt[e] - 1
                toks.append(("c_%s_%d" % (e, i // SEM_ROT), i % SEM_ROT + 1))
            j = self.dcnt[e]
            for jj in range(max(0, j - NDS), j):
                toks.append(("d_%s_%d" % (e, jj % NDS), 16 * (jj // NDS + 1)))
        return toks

    def barrier(self):
        toks = self._all_tokens()
        for e in ENGS:
            for (s, v) in toks:
                if self.waited[e].get(s, 0) >= v:
                    continue
                self.pend[e][s] = max(self.pend[e].get(s, 0), v)
        self.lastw = {}
        self.readers = {}

    def emit(self):
        nc = self.nc
        names = set()
        for e in ENGS:
            for (waits, fn, s, n) in self.ops[e]:
                names.add(s)
                for (ws, wv) in waits:
                    names.add(ws)
        for nm in sorted(names):
            self.sems[nm] = nc.alloc_semaphore(name=nm)
        fin = dict(self._all_tokens())
        for s, v in self.final.items():
            fin[s] = max(fin.get(s, 0), v)
        with nc.Block() as block:
            def body(ename):
                def run(eng):
                    for (waits, fn, s, n) in self.ops[ename]:
                        for (ws, wv) in waits:
                            eng.wait_ge(self.sems[ws], wv)
                        fn(eng).then_inc(self.sems[s], n)
                    if ename == "sp":
                        for s, v in fin.items():
                            eng.wait_ge(self.sems[s], v)
                return run
            block.tensor(body("pe"))
            block.scalar(body("act"))
            block.vector(body("dve"))
            block.gpsimd(body("pool"))
            block.sync(body("sp"))


class Mem:
    BASE = 16512
    TOP = 229344

    def __init__(self, nc):
        self.nc = nc
        self.ptr = self.BASE
        self.persist = self.BASE
        self.n = 0

    def alloc(self, name, shape, dtype):
        esz = 2 if dtype == BF16 else 4
        size = esz
        for d in shape[1:]:
            size *= d
        size = (size + 31) // 32 * 32
        off = self.ptr
        self.ptr += size
        assert self.ptr <= self.TOP, ("SBUF overflow", name, self.ptr)
        self.n += 1
        return self.nc.alloc_sbuf_tensor_at("%s_%d" % (name, self.n), list(shape), dtype, offset=off)

    def keep(self):
        self.persist = self.ptr

    def reset(self):
        self.ptr = self.persist


class Ring:
    def __init__(self, items):
        self.items = items
        self.i = 0

    def next(self):
        it = self.items[self.i % len(self.items)]
        self.i += 1
        return it


PARTS = [("u", 0, 768, "f"), ("v", 768, 768, "t"), ("q", 1536, 768, "f"), ("k", 2304, 768, "f"),
         ("va", 3072, 768, "t"), ("m", 3840, 512, "f")]


def build(upto=99, dbg=()):
    nc = bass.Bass("TRN2", target_bir_lowering=False)
    P = Prog(nc)
    M = Mem(nc)

    def dram_in(name, shape, dt=F32):
        return nc.dram_tensor(name, list(shape), dt, kind="ExternalInput").ap()

    def dram_scr(name, shape, dt):
        kind = "ExternalOutput" if name in dbg else "Internal"
        return nc.dram_tensor(name, list(shape), dt, kind=kind).ap()

    x_d = dram_in("x", [S, D])
    mem_d = dram_in("mem", [256, D])
    win_d = dram_in("w_in_r", [9, 128, 16 * 512])
    wsp_d = dram_in("w_spatial", [6, 128, 128])
    bsp_d = dram_in("b_spatial", [1, 768])
    alng_d = dram_in("a_ln_g", [1, 768])
    alnb_d = dram_in("a_ln_b", [1, 768])
    wkv_d = dram_in("w_kv_r", [2, 128, 16 * 512])
    ga_d = dram_in("norm_a_g", [768])
    gb_d = dram_in("norm_b_g", [768])
    gm_d = dram_in("norm_m_g", [512])
    wout_d = dram_in("w_out_r", [128, 16 * 2048])
    ln1g_d = dram_in("ln1_g", [1, D])
    ln1b_d = dram_in("ln1_b", [1, D])
    wr_d = dram_in("w_router_r", [128, 16 * 32])
    br_d = dram_in("b_router", [1, 32])
    import os as _os2
    NEW = NE if (upto >= 4 and not _os2.environ.get("FORCE_NEW1")) else 1
    wgu_d = dram_in("w_gu_r", [NEW * 8 * 128, 16 * 512])
    bgu_d = dram_in("b_gu_r", [NE * 128, 32])
    wd_d = dram_in("w_d_r", [NEW * 4 * 128, 16 * 512])
    bd_d = dram_in("b_down", [NE, D])
    ln2g_d = dram_in("ln2_g", [1, D])
    ln2b_d = dram_in("ln2_b", [1, D])
    cst_d = dram_in("cst", [128, 417])
    mask_d = dram_in("maskc", [128, 17 * 128])
    out_d = nc.dram_tensor("out", [S, D], F32, kind="ExternalOutput").ap()

    qT_d = dram_scr("qT_s", [6, 128, S], BF16)
    kT_d = dram_scr("kT_s", [6, 128, S], BF16)
    v_d = dram_scr("v_s", [S, 768], BF16)
    qmT_d = dram_scr("qmT_s", [4, 128, S], BF16)
    yT_d = dram_scr("yT_s", [16, 128, S], BF16)
    x1_d = dram_scr("x1_s", [S, D], F32)
    x1b_d = dram_scr("x1b_s", [S, D], BF16)
    xs_d = dram_scr("xs_s", [NBLK * 512, D], BF16)
    ys_d = dram_scr("ys_s", [NBLK * 512, D], F32)
    bexp_d = dram_scr("bexp_s", [128, 1], I32)
    rt_d = dram_scr("rt_s", [128, NT * 8], F32)

    psf = [nc.alloc_psum_tensor("psf%d" % i, [128, 512], F32) for i in range(6)]
    psbs = [nc.alloc_psum_tensor("psb%d" % i, [128, 1024], BF16) for i in range(2)]

    cst = M.alloc("cst", [128, 417], F32)
    identf = cst[:, 0:128]
    iota1 = cst[:, 256:288]
    thr = cst[:, 288:289]
    identb = M.alloc("identb", [128, 128], BF16)
    trib = M.alloc("trib", [128, 128], BF16)
    onesb = M.alloc("onesb", [128, 128], BF16)
    onesrow = M.alloc("onesrow", [1, 512], BF16)
    onesrow32 = M.alloc("onesrow32", [1, 128], F32)
    KmT = M.alloc("KmT", [128, 4 * 256], BF16)
    Vm = M.alloc("Vm", [128, 2 * 512], BF16)
    gA = M.alloc("gA", [128, 6], F32)
    gB = M.alloc("gB", [128, 6], F32)
    gM = M.alloc("gM", [128, 4], F32)
    rank_all = M.alloc("rank_all", [128, NT * 32], F32)
    sel_all = M.alloc("sel_all", [128, NT * 32], F32)
    gate_all = M.alloc("gate_all", [128, NT * 32], F32)
    dest4i = M.alloc("dest4i", [128, NT * 4], I32)
    gate4 = M.alloc("gate4", [128, NT * 4], F32)
    idx_gu = M.alloc("idx_gu", [128, 8 * 64], I32)
    idx_d = M.alloc("idx_d", [128, 4 * 64], I32)
    idx_b = M.alloc("idx_b", [128, 64], I32)
    idx_e = M.alloc("idx_e", [128, 64], I32)
    M.keep()

    P.dma("sp", lambda e: e.dma_start(out=cst[:], in_=cst_d), writes=["cst"])
    P.dma("sp", lambda e: e.dma_start(out=gA[:], in_=ga_d.rearrange("(h p) -> p h", p=128), allow_slow_non_contiguous=True), writes=["gA"])
    P.dma("sp", lambda e: e.dma_start(out=gB[:], in_=gb_d.rearrange("(h p) -> p h", p=128), allow_slow_non_contiguous=True), writes=["gB"])
    P.dma("sp", lambda e: e.dma_start(out=gM[:], in_=gm_d.rearrange("(h p) -> p h", p=128), allow_slow_non_contiguous=True), writes=["gM"])
    P.op("dve", lambda e: e.tensor_copy(out=identb[:], in_=cst[:, 0:128]), reads=["cst"], writes=["identb"])
    P.op("dve", lambda e: e.tensor_copy(out=trib[:], in_=cst[:, 128:256]), reads=["cst"], writes=["trib"])
    P.op("dve", lambda e: e.tensor_copy(out=onesb[:], in_=cst[:, 289:417]), reads=["cst"], writes=["onesb"])
    P.op("dve", lambda e: e.memset(onesrow[:], 1.0), writes=["onesrow"])
    P.op("dve", lambda e: e.memset(onesrow32[:], 1.0), writes=["onesrow32"])

    import os as _os
    _EV = _os.environ.get("EVAC", "")

    _bc = {}

    def get_bc(e):
        if "v" not in _bc:
            r = e.alloc_register("bnd")
            e.reg_mov(r, NBLK * 512 - 1)
            _bc["v"] = e.snap(r)
        return _bc["v"]

    def evac(i, **kw):
        if _EV:
            return _EV
        return "act" if i % 2 == 0 else "dve"

    def copy_op(eng, out, in_, reads, writes):
        if eng == "act":
            P.op("act", lambda e: e.activation(out=out, in_=in_, func=AF.Copy), reads=reads, writes=writes)
        else:
            P.op(eng, lambda e: e.tensor_copy(out=out, in_=in_), reads=reads, writes=writes)

    def rstd_rep(pss_key, pss_ap, nfeat, tmp, rrep, key):
        P.op("act", lambda e: e.activation(out=tmp, in_=pss_ap, func=AF.Sqrt, bias=LN_EPS, scale=1.0 / nfeat),
             reads=[pss_key], writes=[key + "_t"])
        P.op("dve", lambda e: e.reciprocal(out=rrep, in_=tmp), reads=[key + "_t"], writes=[key])

    def layer_norm_rows(h, hkeys, gb_t, bb_t, out, outkey, stats, mv, tmp2, width, pfx):
        nch = width // 512 if width % 512 == 0 else 2
        cw = width // nch
        for c in range(nch):
            P.op("dve", lambda e, c=c: e.bn_stats(out=stats[:, c * 6:(c + 1) * 6], in_=h[:, c * cw:(c + 1) * cw]),
                 reads=hkeys, writes=[pfx + "st%d" % c])
        P.op("dve", lambda e: e.bn_aggr(out=mv[:, 0:2], in_=stats[:, 0:nch * 6]),
             reads=[pfx + "st%d" % c for c in range(nch)], writes=[pfx + "mv"])
        P.op("act", lambda e: e.activation(out=tmp2[:, 0:1], in_=mv[:, 1:2], func=AF.Sqrt, bias=LN_EPS, scale=1.0),
             reads=[pfx + "mv"], writes=[pfx + "sd"])
        P.op("dve", lambda e: e.reciprocal(out=tmp2[:, 1:2], in_=tmp2[:, 0:1]), reads=[pfx + "sd"], writes=[pfx + "rs"])
        P.op("dve", lambda e: e.scalar_tensor_tensor(out=h, in0=h, scalar=mv[:, 0:1], in1=gb_t, op0=ALU.subtract, op1=ALU.mult),
             reads=list(hkeys) + [pfx + "mv", "lnconst_a"], writes=list(hkeys))
        P.op("dve", lambda e: e.scalar_tensor_tensor(out=out, in0=h, scalar=tmp2[:, 1:2], in1=bb_t, op0=ALU.mult, op1=ALU.add),
             reads=list(hkeys) + [pfx + "rs", "lnconst_b"], writes=[outkey])

    if upto >= 1:
        WmT = M.alloc("WmT", [128, 6 * 128], BF16)
        bsp = M.alloc("bsp", [1, 768], BF16)
        alng = M.alloc("alng", [128, 768], F32)
        alnb = M.alloc("alnb", [128, 768], F32)
        wspf = M.alloc("wspf", [128, 6 * 128], F32)
        wspb = M.alloc("wspb", [128, 6 * 128], BF16)
        xb = [M.alloc("xb%d" % i, [128, 4 * D], BF16) for i in range(2)]
        xT = M.alloc("xT", [128, 16 * 512], BF16)
        wch = [M.alloc("wch%d" % i, [128, 16 * 512], BF16) for i in range(3)]
        uT = M.alloc("uT", [128, 6 * 512], BF16)
        vf = M.alloc("vf", [128, 4 * 768], F32)
        vn = M.alloc("vn", [128, 4 * 768], BF16)
        vast = M.alloc("vast", [128, 4 * 768], BF16)
        fst = [M.alloc("fst%d" % i, [128, 512], BF16) for i in range(4)]
        yaT = M.alloc("yaT", [128, 6 * 512], BF16)
        sqA = M.alloc("sqA", [128, 6 * 512], BF16)
        yan = M.alloc("yan", [128, 6 * 512], BF16)
        rtmp = M.alloc("rtmp", [128, 512], F32)
        rrep = M.alloc("rrep", [128, 512], F32)
        stats = M.alloc("stats", [128, 24], F32)
        mv = M.alloc("mv", [128, 2], F32)
        tmp2 = M.alloc("tmp2", [128, 2], F32)

        P.dma("sp", lambda e: e.dma_start(out=wspf[:].rearrange("p (g s) -> p g s", g=6), in_=wsp_d.rearrange("g t s -> t g s")), writes=["wspf"])
        P.dma("pool", lambda e: e.dma_start(out=bsp[:], in_=bsp_d), writes=["bsp"])
        P.dma("sp", lambda e: e.dma_start(out=alng[:], in_=alng_d.partition_broadcast(128)), writes=["lnconst_a"])
        P.dma("sp", lambda e: e.dma_start(out=alnb[:], in_=alnb_d.partition_broadcast(128)), writes=["lnconst_b"])
        for g in range(6):
            P.op("pool", lambda e, g=g: e.affine_select(out=wspf[:, g * 128:(g + 1) * 128], in_=wspf[:, g * 128:(g + 1) * 128],
                                                        pattern=[[-1, 128]], compare_op=ALU.is_ge, fill=0.0, base=0,
                                                        channel_multiplier=1), reads=["wspf"], writes=["wspf"])
        P.op("dve", lambda e: e.tensor_copy(out=wspb[:], in_=wspf[:]), reads=["wspf"], writes=["wspb"])
        for g in range(6):
            P.op("pe", lambda e, g=g: e.transpose(out=psbs[0][:, g * 128:(g + 1) * 128], in_=wspb[:, g * 128:(g + 1) * 128], identity=identb[:]),
                 reads=["wspb", "identb"], writes=["psb0"])
        P.op("dve", lambda e: e.tensor_copy(out=WmT[:], in_=psbs[0][:, 0:768]), reads=["psb0"], writes=["WmT"])

        psA = Ring([0, 1, 2])
        psM = Ring([3, 4])
        fstR = Ring([0, 1, 2, 3])
        loads = [(G, c) for G in range(NG) for c in range(9)]
        nload = [0]
        ev = [0]

        def issue_loads(upto_idx):
            while nload[0] <= upto_idx and nload[0] < len(loads):
                G, c = loads[nload[0]]
                if c == 0:
                    b = G % 2
                    for jx in range(4):
                        P.dma("pool", lambda e, G=G, b=b, jx=jx: e.dma_start(
                            out=xb[b][:, jx * D:(jx + 1) * D],
                            in_=x_d[G * GT + jx * 128: G * GT + (jx + 1) * 128, :]), writes=["xb%d_%d" % (b, jx)])
                i = nload[0] % 3
                P.dma("pool", lambda e, c=c, i=i: e.dma_start(out=wch[i][:], in_=win_d[c]), writes=["wch%d" % i])
                nload[0] += 1

        import os
        P1_NG = int(os.environ.get("P1_NG", NG))
        P1_NC = int(os.environ.get("P1_NC", 9))
        P1_TR = int(os.environ.get("P1_TR", 16))
        for G in range(P1_NG):
            gsl = slice(G * GT, (G + 1) * GT)
            b = G % 2
            issue_loads(G * 9 + 1)
            for kc in range(P1_TR):
                hb = kc % 2
                for j in range(4):
                    P.op("pe", lambda e, kc=kc, j=j, hb=hb, b=b: e.transpose(
                        out=psbs[hb][:, j * 128:(j + 1) * 128],
                        in_=xb[b][:, j * D + kc * 128: j * D + (kc + 1) * 128], identity=identb[:]),
                        reads=["xb%d_%d" % (b, j), "identb"], writes=["psb%d" % hb])
                copy_op(evac(kc), xT[:, kc * 512:(kc + 1) * 512], psbs[hb][:, 0:512], ["psb%d" % hb], ["xT"])
            for c in range(P1_NC):
                li = G * 9 + c
                issue_loads(li + 2)
                wi = li % 3
                wt = wch[wi]
                wkey = "wch%d" % wi
                c0 = c * 512
                for (pname, pstart, pwidth, lay) in PARTS:
                    lo = max(pstart, c0)
                    hi = min(pstart + pwidth, c0 + 512)
                    if lo >= hi:
                        continue
                    if lay == "f":
                        for blk in range((lo - pstart) // 128, (hi - pstart) // 128):
                            off = pstart + blk * 128 - c0
                            pi = psA.next()
                            for kc in range(16):
                                P.op("pe", lambda e, pi=pi, kc=kc, off=off, wt=wt: e.matmul(
                                    psf[pi][:, 0:512], lhsT=wt[:, kc * 512 + off: kc * 512 + off + 128],
                                    rhs=xT[:, kc * 512:(kc + 1) * 512], start=(kc == 0), stop=(kc == 15)),
                                    reads=[wkey, "xT"], writes=["psf%d" % pi])
                            if pname == "u":
                                P.op("act", lambda e, pi=pi, blk=blk: e.activation(
                                    out=uT[:, blk * 512:(blk + 1) * 512], in_=psf[pi][:, 0:512], func=AF.Gelu_apprx_tanh),
                                    reads=["psf%d" % pi], writes=["uT%d" % blk])
                            else:
                                fi = fstR.next()
                                ev[0] += 1
                                copy_op(evac(ev[0]), fst[fi][:], psf[pi][:, 0:512], ["psf%d" % pi], ["fst%d" % fi])
                                dst = {"q": qT_d, "k": kT_d, "m": qmT_d}[pname]
                                P.dma("sp", lambda e, dst=dst, blk=blk, fi=fi, gsl=gsl: e.dma_start(out=dst[blk, :, gsl], in_=fst[fi][:]),
                                      reads=["fst%d" % fi], writes=[pname + "_d"])
                    else:
                        wd = hi - lo
                        off = lo - c0
                        oc = lo - pstart
                        for j in range(4):
                            pi = psA.next()
                            for kc in range(16):
                                P.op("pe", lambda e, pi=pi, kc=kc, off=off, wd=wd, j=j, wt=wt: e.matmul(
                                    psf[pi][:, 0:wd], lhsT=xT[:, kc * 512 + j * 128: kc * 512 + (j + 1) * 128],
                                    rhs=wt[:, kc * 512 + off: kc * 512 + off + wd], start=(kc == 0), stop=(kc == 15)),
                                    reads=[wkey, "xT"], writes=["psf%d" % pi])
                            if pname == "v":
                                P.op("act", lambda e, pi=pi, j=j, oc=oc, wd=wd: e.activation(
                                    out=vf[:, j * 768 + oc: j * 768 + oc + wd], in_=psf[pi][:, 0:wd], func=AF.Gelu_apprx_tanh),
                                    reads=["psf%d" % pi], writes=["vf%d_%d" % (j, oc)])
                            else:
                                ev[0] += 1
                                copy_op(evac(ev[0]), vast[:, j * 768 + oc: j * 768 + oc + wd], psf[pi][:, 0:wd],
                                        ["psf%d" % pi], ["vast%d_%d" % (j, oc)])
                                if oc + wd == 768:
                                    P.dma("sp", lambda e, j=j, G=G: e.dma_start(
                                        out=v_d[G * GT + j * 128: G * GT + (j + 1) * 128, :], in_=vast[:, j * 768:(j + 1) * 768]),
                                        reads=["vast%d_0" % j, "vast%d_512" % j], writes=["v_d"])
                if c == 2:
                    for j in range(4):
                        hj = vf[:, j * 768:(j + 1) * 768]
                        layer_norm_rows(hj, ["vf%d_0" % j, "vf%d_256" % j], alng[:], alnb[:], vn[:, j * 768:(j + 1) * 768], "vn%d" % j,
                                        stats, mv, tmp2, 768, "g_")
                    for g in range(6):
                        pi = psM.next()
                        for j in range(4):
                            P.op("pe", lambda e, pi=pi, j=j, g=g: e.matmul(
                                psf[pi][:, j * 128:(j + 1) * 128], lhsT=vn[:, j * 768 + g * 128: j * 768 + (g + 1) * 128],
                                rhs=WmT[:, g * 128:(g + 1) * 128], start=True, stop=False),
                                reads=["vn%d" % j, "WmT"], writes=["psf%d" % pi])
                            P.op("pe", lambda e, pi=pi, j=j, g=g: e.matmul(
                                psf[pi][:, j * 128:(j + 1) * 128], lhsT=onesrow[0:1, 0:128],
                                rhs=bsp[0:1, g * 128:(g + 1) * 128], start=False, stop=True),
                                reads=["onesrow", "bsp"], writes=["psf%d" % pi])
                        P.op("dve", lambda e, pi=pi, g=g: e.tensor_tensor(
                            out=yaT[:, g * 512:(g + 1) * 512], in0=psf[pi][:, 0:512], in1=uT[:, g * 512:(g + 1) * 512], op=ALU.mult),
                            reads=["psf%d" % pi, "uT%d" % g], writes=["yaT"])
                    P.op("act", lambda e: e.activation(out=sqA[:], in_=yaT[:], func=AF.Square), reads=["yaT"], writes=["sqA"])
                    for g in range(6):
                        P.op("pe", lambda e, g=g: e.matmul(psf[5][:, 0:512], lhsT=onesb[:], rhs=sqA[:, g * 512:(g + 1) * 512],
                                                           start=(g == 0), stop=(g == 5)), reads=["sqA", "onesb"], writes=["psf5"])
                    rstd_rep("psf5", psf[5][:, 0:512], 768.0, rtmp[:], rrep[:], "rrepA")
                    for g in range(6):
                        P.op("dve", lambda e, g=g: e.scalar_tensor_tensor(
                            out=yan[:, g * 512:(g + 1) * 512], in0=yaT[:, g * 512:(g + 1) * 512], scalar=gA[:, g:g + 1],
                            in1=rrep[:], op0=ALU.mult, op1=ALU.mult), reads=["yaT", "gA", "rrepA"], writes=["yan"])
                    P.dma("sp", lambda e, gsl=gsl: e.dma_start(out=yT_d[0:6, :, gsl].rearrange("g p t -> p g t"),
                                                             in_=yan[:].rearrange("p (g t) -> p g t", g=6)),
                          reads=["yan"], writes=["yT_d"])
        P.barrier()
        M.reset()

    if upto >= 2:
        memb = M.alloc("memb", [128, 2 * D], BF16)
        memT = M.alloc("memT", [128, 16 * 256], BF16)
        wkv = [M.alloc("wkv%d" % i, [128, 16 * 512], BF16) for i in range(2)]
        for jx in range(2):
            P.dma("pool", lambda e, jx=jx: e.dma_start(out=memb[:, jx * D:(jx + 1) * D], in_=mem_d[jx * 128:(jx + 1) * 128, :]),
                  writes=["memb%d" % jx])
        for i in range(2):
            P.dma("pool", lambda e, i=i: e.dma_start(out=wkv[i][:], in_=wkv_d[i]), writes=["wkv%d" % i])
        for kc in range(16):
            hb = kc % 2
            for j in range(2):
                P.op("pe", lambda e, kc=kc, j=j, hb=hb: e.transpose(
                    out=psbs[hb][:, j * 128:(j + 1) * 128],
                    in_=memb[:, j * D + kc * 128: j * D + (kc + 1) * 128], identity=identb[:]),
                    reads=["memb%d" % j, "identb"], writes=["psb%d" % hb])
            copy_op(evac(kc), memT[:, kc * 256:(kc + 1) * 256], psbs[hb][:, 0:256], ["psb%d" % hb], ["memT"])
        for h in range(4):
            pi = h % 4
            for kc in range(16):
                P.op("pe", lambda e, pi=pi, kc=kc, h=h: e.matmul(
                    psf[pi][:, 0:256], lhsT=wkv[0][:, kc * 512 + h * 128: kc * 512 + (h + 1) * 128],
                    rhs=memT[:, kc * 256:(kc + 1) * 256], start=(kc == 0), stop=(kc == 15)),
                    reads=["wkv0", "memT"], writes=["psf%d" % pi])
            copy_op(evac(h), KmT[:, h * 256:(h + 1) * 256], psf[pi][:, 0:256], ["psf%d" % pi], ["KmT"])
        for blk in range(2):
            pi = 4 + blk
            for kc in range(16):
                P.op("pe", lambda e, pi=pi, kc=kc, blk=blk: e.matmul(
                    psf[pi][:, 0:512], lhsT=memT[:, kc * 256 + blk * 128: kc * 256 + (blk + 1) * 128],
                    rhs=wkv[1][:, kc * 512:(kc + 1) * 512], start=(kc == 0), stop=(kc == 15)),
                    reads=["wkv1", "memT"], writes=["psf%d" % pi])
            copy_op(evac(blk), Vm[:, blk * 512:(blk + 1) * 512], psf[pi][:, 0:512], ["psf%d" % pi], ["Vm"])
        P.barrier()
        M.reset()

        maskb = M.alloc("maskb", [128, 17 * 128], BF16)
        KT = M.alloc("KT", [128, 6 * S], BF16)
        Vt = M.alloc("Vt", [128, NT * 768], BF16)
        QT = [M.alloc("QT%d" % i, [128, 6 * 512], BF16) for i in range(2)]
        QmT = [M.alloc("QmT%d" % i, [128, 4 * 512], BF16) for i in range(2)]
        pTb = [M.alloc("pTb%d" % i, [128, 512], BF16) for i in range(3)]
        rd = [M.alloc("rd%d" % i, [128, 512], F32) for i in range(2)]
        ybT = M.alloc("ybT", [128, 6 * 512], BF16)
        ymT = M.alloc("ymT", [128, 4 * 512], BF16)
        sqB = M.alloc("sqB", [128, 6 * 512], BF16)
        ybn = M.alloc("ybn", [128, 6 * 512], BF16)
        rtmp3 = M.alloc("rtmp3", [128, 512], F32)
        rrep3 = M.alloc("rrep3", [128, 512], F32)

        P.dma("pool", lambda e: e.dma_start(out=maskb[:], in_=mask_d), writes=["maskb"])
        for h in range(6):
            P.dma("sp", lambda e, h=h: e.dma_start(out=KT[:, h * S:(h + 1) * S], in_=kT_d[h]), writes=["KT%d" % h])
        for q4 in range(4):
            P.dma("sp", lambda e, q4=q4: e.dma_start(
                out=Vt[:, q4 * 8 * 768:(q4 + 1) * 8 * 768].rearrange("p (b c) -> p b c", b=8),
                in_=v_d[q4 * 1024:(q4 + 1) * 1024, :].rearrange("(b p) c -> p b c", p=128)), writes=["Vt%d" % q4])

        psS = Ring([0, 1])
        psO = Ring([2, 3])
        psD = Ring([4, 5])
        pTR = Ring([0, 1, 2])
        rdR = Ring([0, 1])

        def load_q(G):
            b = G % 2
            gsl = slice(G * GT, (G + 1) * GT)
            P.dma("sp", lambda e: e.dma_start(out=QT[b][:].rearrange("p (h t) -> p h t", h=6),
                                              in_=qT_d[:, :, gsl].rearrange("h p t -> p h t")), writes=["QT%d" % b])
            P.dma("sp", lambda e: e.dma_start(out=QmT[b][:].rearrange("p (h t) -> p h t", h=4),
                                              in_=qmT_d[:, :, gsl].rearrange("h p t -> p h t")), writes=["QmT%d" % b])

        def rms_store(yT, ykey, nh, gains, gkey, nfeat, cbase, gsl):
            P.op("act", lambda e: e.activation(out=sqB[:, 0:nh * 512], in_=yT[:, 0:nh * 512], func=AF.Square),
                 reads=[ykey], writes=["sqB"])
            for g in range(nh):
                P.op("pe", lambda e, g=g: e.matmul(psf[0][:, 0:512], lhsT=onesb[:], rhs=sqB[:, g * 512:(g + 1) * 512],
                                                   start=(g == 0), stop=(g == nh - 1)), reads=["sqB", "onesb"], writes=["psf0"])
            rstd_rep("psf0", psf[0][:, 0:512], float(nfeat), rtmp3[:], rrep3[:], "rrepB")
            for g in range(nh):
                P.op("dve", lambda e, g=g: e.scalar_tensor_tensor(
                    out=ybn[:, g * 512:(g + 1) * 512], in0=yT[:, g * 512:(g + 1) * 512], scalar=gains[:, g:g + 1],
                    in1=rrep3[:], op0=ALU.mult, op1=ALU.mult), reads=[ykey, gkey, "rrepB"], writes=["ybn"])
            P.dma("sp", lambda e: e.dma_start(out=yT_d[cbase:cbase + nh, :, gsl].rearrange("g p t -> p g t"),
                                              in_=ybn[:, 0:nh * 512].rearrange("p (g t) -> p g t", g=nh)),
                  reads=["ybn"], writes=["yT_d"])

        load_q(0)
        for G in range(NG):
            b = G % 2
            gsl = slice(G * GT, (G + 1) * GT)
            if G + 1 < NG:
                load_q(G + 1)
            for hh in range(10):
                isB = hh < 6
                h = hh if isB else hh - 6
                po = psO.next()
                pd = psD.next()
                for qi in range(4):
                    n = 4 * G + qi
                    if isB:
                        kbs = list(range(max(0, n - 16), n + 1))
                        qap = QT[b][:, h * 512 + qi * 128: h * 512 + (qi + 1) * 128]
                        qkey = "QT%d" % b
                    else:
                        kbs = [0, 1]
                        qap = QmT[b][:, h * 512 + qi * 128: h * 512 + (qi + 1) * 128]
                        qkey = "QmT%d" % b
                    for c0 in range(0, len(kbs), 4):
                        ch = kbs[c0:c0 + 4]
                        L = len(ch)
                        ps = psS.next()
                        for jj, kb in enumerate(ch):
                            if isB:
                                kap = KT[:, h * S + kb * 128: h * S + (kb + 1) * 128]
                                kkey = "KT%d" % h
                            else:
                                kap = KmT[:, h * 256 + kb * 128: h * 256 + (kb + 1) * 128]
                                kkey = "KmT"
                            P.op("pe", lambda e, ps=ps, jj=jj, kap=kap, qap=qap: e.matmul(
                                psf[ps][:, jj * 128:(jj + 1) * 128], lhsT=kap, rhs=qap, start=True, stop=True),
                                reads=[kkey, qkey], writes=["psf%d" % ps])
                        pt = pTR.next()
                        P.op("act", lambda e, ps=ps, pt=pt, L=L: e.activation(
                            out=pTb[pt][:, 0:L * 128], in_=psf[ps][:, 0:L * 128], func=AF.Exp, scale=SCALE),
                            reads=["psf%d" % ps], writes=["pTb%d" % pt])
                        if isB:
                            i0 = ch[0] - (n - 16)
                            P.op("dve", lambda e, pt=pt, L=L, i0=i0: e.tensor_tensor(
                                out=pTb[pt][:, 0:L * 128], in0=pTb[pt][:, 0:L * 128], in1=maskb[:, i0 * 128:(i0 + L) * 128], op=ALU.mult),
                                reads=["pTb%d" % pt, "maskb"], writes=["pTb%d" % pt])
                        for jj, kb in enumerate(ch):
                            first = (c0 + jj == 0)
                            last = (c0 + jj == len(kbs) - 1)
                            if isB:
                                vap = Vt[:, kb * 768 + h * 128: kb * 768 + (h + 1) * 128]
                                vkey = "Vt%d" % (kb // 8)
                            else:
                                vap = Vm[:, kb * 512 + h * 128: kb * 512 + (h + 1) * 128]
                                vkey = "Vm"
                            P.op("pe", lambda e, po=po, qi=qi, vap=vap, pt=pt, jj=jj, first=first, last=last: e.matmul(
                                psf[po][:, qi * 128:(qi + 1) * 128], lhsT=vap, rhs=pTb[pt][:, jj * 128:(jj + 1) * 128],
                                start=first, stop=last), reads=[vkey, "pTb%d" % pt], writes=["psf%d" % po])
                            P.op("pe", lambda e, pd=pd, qi=qi, pt=pt, jj=jj, first=first, last=last: e.matmul(
                                psf[pd][:, qi * 128:(qi + 1) * 128], lhsT=onesb[:], rhs=pTb[pt][:, jj * 128:(jj + 1) * 128],
                                start=first, stop=last), reads=["onesb", "pTb%d" % pt], writes=["psf%d" % pd])
                ri = rdR.next()
                P.op("dve", lambda e, pd=pd, ri=ri: e.reciprocal(out=rd[ri][:], in_=psf[pd][:, 0:512]),
                     reads=["psf%d" % pd], writes=["rd%d" % ri])
                ydst = ybT if isB else ymT
                ykey = "ybT" if isB else "ymT"
                P.op("dve", lambda e, po=po, ri=ri, ydst=ydst, h=h: e.tensor_tensor(
                    out=ydst[:, h * 512:(h + 1) * 512], in0=psf[po][:, 0:512], in1=rd[ri][:], op=ALU.mult),
                    reads=["psf%d" % po, "rd%d" % ri], writes=[ykey])
                if hh == 5:
                    rms_store(ybT, "ybT", 6, gB, "gB", 768, 6, gsl)
                if hh == 9:
                    rms_store(ymT, "ymT", 4, gM, "gM", 512, 12, gsl)
        P.barrier()
        M.reset()

    if upto >= 3:
        wout = M.alloc("wout", [128, 16 * D], BF16)
        lng = M.alloc("lng", [128, D], F32)
        lnb = M.alloc("lnb", [128, D], F32)
        wr = M.alloc("wr", [128, 16 * 32], F32)
        br = M.alloc("br", [1, 32], F32)
        tot = M.alloc("tot", [128, 32], F32)
        yTt = [M.alloc("yTt%d" % i, [128, 16 * 512], BF16) for i in range(2)]
        xt = [M.alloc("xt%d" % i, [128, D], F32) for i in range(2)]
        hb_ = xt
        x1t = [M.alloc("x1t%d" % i, [128, D], F32) for i in range(2)]
        x1b = [M.alloc("x1b%d" % i, [128, D], BF16) for i in range(2)]
        x1T = M.alloc("x1T", [128, 16 * 128], F32)
        stats = M.alloc("stats4", [128, 24], F32)
        mv = M.alloc("mv4", [128, 2], F32)
        tmp2 = M.alloc("tmp24", [128, 2], F32)
        lg = M.alloc("lg", [128, 32], F32)
        m8 = M.alloc("m8", [128, 8], F32)
        negm = M.alloc("negm", [128, 1], F32)
        ex = M.alloc("ex", [128, 32], F32)
        den = M.alloc("den", [128, 2], F32)
        selb = M.alloc("selb", [128, 32], BF16)

        for q4 in range(4):
            P.dma("pool", lambda e, q4=q4: e.dma_start(out=wout[:, q4 * 4 * D:(q4 + 1) * 4 * D],
                                                       in_=wout_d[:, q4 * 4 * D:(q4 + 1) * 4 * D]), writes=["wout"])
        P.dma("sp", lambda e, t=lng: e.dma_start(out=t[:], in_=ln1g_d.partition_broadcast(128)), writes=["lnconst_a"])
        P.dma("sp", lambda e, t=lnb: e.dma_start(out=t[:], in_=ln1b_d.partition_broadcast(128)), writes=["lnconst_b"])
        P.dma("sp", lambda e: e.dma_start(out=wr[:], in_=wr_d), writes=["wr"])
        P.dma("sp", lambda e: e.dma_start(out=br[:], in_=br_d), writes=["br"])
        P.op("dve", lambda e: e.memset(tot[:], 0.0), writes=["tot"])

        psX = Ring([0, 1, 2])
        psT = Ring([3, 4])

        def load_y(G):
            b = G % 2
            gsl = slice(G * GT, (G + 1) * GT)
            P.dma("sp", lambda e: e.dma_start(out=yTt[b][:].rearrange("p (c t) -> p c t", c=16),
                                              in_=yT_d[:, :, gsl].rearrange("c p t -> p c t")), writes=["yTt%d" % b])

        def load_x(T):
            b = T % 2
            P.dma("sp", lambda e: e.dma_start(out=xt[b][:], in_=x_d[T * 128:(T + 1) * 128, :]),
                  writes=["xt%d" % b] + ["hb%d_%d" % (b, cc) for cc in range(4)])

        load_y(0)
        load_x(0)
        for T in range(NT):
            G, j = T // 4, T % 4
            b = T % 2
            yb = G % 2
            if j == 0 and G + 1 < NG:
                load_y(G + 1)
            if T + 1 < NT:
                load_x(T + 1)
            hk = "hb%d" % b
            for cc in range(4):
                pi = psX.next()
                for fc in range(16):
                    P.op("pe", lambda e, pi=pi, fc=fc, cc=cc, j=j, yb=yb: e.matmul(
                        psf[pi][:, 0:512], lhsT=yTt[yb][:, fc * 512 + j * 128: fc * 512 + (j + 1) * 128],
                        rhs=wout[:, fc * D + cc * 512: fc * D + (cc + 1) * 512], start=(fc == 0), stop=(fc == 15)),
                        reads=["yTt%d" % yb, "wout"], writes=["psf%d" % pi])
                P.op("dve", lambda e, pi=pi, cc=cc, b=b: e.scalar_tensor_tensor(
                    out=hb_[b][:, cc * 512:(cc + 1) * 512], in0=xt[b][:, cc * 512:(cc + 1) * 512], scalar=DN_ALPHA,
                    in1=psf[pi][:, 0:512], op0=ALU.mult, op1=ALU.add), reads=["xt%d" % b, "psf%d" % pi], writes=[hk + "_%d" % cc])
            layer_norm_rows(hb_[b][:], [hk + "_%d" % cc for cc in range(4)], lng[:], lnb[:], x1t[b][:], "x1t%d" % b, stats, mv, tmp2, D, "l1_")
            P.dma("sp", lambda e, T=T, b=b: e.dma_start(out=x1_d[T * 128:(T + 1) * 128, :], in_=x1t[b][:]),
                  reads=["x1t%d" % b], writes=["x1_d"])
            P.op("act", lambda e, b=b: e.activation(out=x1b[b][:], in_=x1t[b][:], func=AF.Copy),
                 reads=["x1t%d" % b], writes=["x1b%d" % b])
            P.dma("sp", lambda e, T=T, b=b: e.dma_start(out=x1b_d[T * 128:(T + 1) * 128, :], in_=x1b[b][:]),
                  reads=["x1b%d" % b], writes=["x1b_d"])
            for k4 in range(4):
                pi = psT.next()
                for kk in range(4):
                    kc = k4 * 4 + kk
                    P.op("pe", lambda e, pi=pi, kk=kk, kc=kc, b=b: e.transpose(
                        out=psf[pi][:, kk * 128:(kk + 1) * 128], in_=x1t[b][:, kc * 128:(kc + 1) * 128], identity=identf),
                        reads=["x1t%d" % b, "cst"], writes=["psf%d" % pi])
                copy_op("act", x1T[:, k4 * 512:(k4 + 1) * 512], psf[pi][:, 0:512], ["psf%d" % pi], ["x1T"])
            for kc in range(16):
                P.op("pe", lambda e, kc=kc: e.matmul(psf[5][:, 0:32], lhsT=x1T[:, kc * 128:(kc + 1) * 128],
                                                     rhs=wr[:, kc * 32:(kc + 1) * 32], start=(kc == 0), stop=False),
                     reads=["x1T", "wr"], writes=["psf5"])
            P.op("pe", lambda e: e.matmul(psf[5][:, 0:32], lhsT=onesrow32[0:1, 0:128], rhs=br[0:1, :], start=False, stop=True),
                 reads=["onesrow32", "br"], writes=["psf5"])
            tsl = slice(T * 32, (T + 1) * 32)
            P.op("dve", lambda e: e.tensor_copy(out=lg[:], in_=psf[5][:, 0:32]), reads=["psf5"], writes=["lg"])
            P.op("dve", lambda e: e.max(out=m8[:], in_=lg[:]), reads=["lg"], writes=["m8"])
            P.op("dve", lambda e, tsl=tsl: e.tensor_single_scalar(out=sel_all[:, tsl], in_=lg[:], scalar=m8[:, 3:4], op=ALU.is_ge),
                 reads=["lg", "m8"], writes=["sel"])
            P.op("dve", lambda e: e.tensor_single_scalar(out=negm[:], in_=m8[:, 0:1], scalar=-1.0, op=ALU.mult), reads=["m8"], writes=["negm"])
            P.op("act", lambda e: e.activation(out=ex[:], in_=lg[:], func=AF.Exp, bias=negm[:, 0:1], scale=1.0),
                 reads=["lg", "negm"], writes=["ex"])
            P.op("dve", lambda e, tsl=tsl: e.tensor_tensor(out=ex[:], in0=ex[:], in1=sel_all[:, tsl], op=ALU.mult),
                 reads=["ex", "sel"], writes=["ex"])
            P.op("dve", lambda e: e.reduce_sum(out=den[:, 0:1], in_=ex[:], axis=AX.X), reads=["ex"], writes=["den"])
            P.op("dve", lambda e: e.reciprocal(out=den[:, 1:2], in_=den[:, 0:1]), reads=["den"], writes=["rden"])
            P.op("dve", lambda e, tsl=tsl: e.tensor_single_scalar(out=gate_all[:, tsl], in_=ex[:], scalar=den[:, 1:2], op=ALU.mult),
                 reads=["ex", "rden"], writes=["gate"])
            P.op("dve", lambda e, tsl=tsl: e.tensor_copy(out=selb[:], in_=sel_all[:, tsl]), reads=["sel"], writes=["selb"])
            P.op("pe", lambda e: e.matmul(psf[5][:, 64:96], lhsT=trib[:], rhs=selb[:], start=True, stop=True),
                 reads=["trib", "selb", "lg"], writes=["psf5"])
            P.op("pe", lambda e: e.matmul(psf[5][:, 96:128], lhsT=onesb[:], rhs=selb[:], start=True, stop=True),
                 reads=["onesb", "selb"], writes=["psf5"])
            P.op("dve", lambda e, tsl=tsl: e.tensor_tensor(out=rank_all[:, tsl], in0=psf[5][:, 64:96], in1=tot[:], op=ALU.add),
                 reads=["psf5", "tot"], writes=["rank"])
            P.op("dve", lambda e: e.tensor_tensor(out=tot[:], in0=psf[5][:, 96:128], in1=tot[:], op=ALU.add),
                 reads=["psf5", "tot"], writes=["tot", "psf5"])

        ci = M.alloc("ci", [128, 32], I32)
        padded = M.alloc("padded", [128, 32], F32)
        csA = M.alloc("csA", [128, 32], F32)
        csB = M.alloc("csB", [128, 32], F32)
        pstart = M.alloc("pstart", [128, 32], F32)
        cmpt = M.alloc("cmpt", [128, 32], F32)
        bex = M.alloc("bex", [128, 1], F32)
        bexi = M.alloc("bexi", [128, 1], I32)
        dst = M.alloc("dst", [128, 32], F32)
        m8d = M.alloc("m8d", [128, 8], F32)
        key2 = M.alloc("key2", [128, 32], F32)
        m8e = M.alloc("m8e", [128, 8], F32)
        tq = M.alloc("tq", [128, 32], F32)
        xsc = [M.alloc("xsc%d" % i, [128, D], BF16) for i in range(3)]

        P.op("dve", lambda e: e.tensor_copy(out=ci[:], in_=tot[:]), reads=["tot"], writes=["ci"])
        P.op("dve", lambda e: e.tensor_single_scalar(out=ci[:], in_=ci[:], scalar=511, op=ALU.add), reads=["ci"], writes=["ci"])
        P.op("dve", lambda e: e.tensor_scalar(out=ci[:], in0=ci[:], scalar1=9, scalar2=9, op0=ALU.arith_shift_right,
                                              op1=ALU.logical_shift_left), reads=["ci"], writes=["ci"])
        P.op("dve", lambda e: e.tensor_copy(out=padded[:], in_=ci[:]), reads=["ci"], writes=["padded"])
        P.op("dve", lambda e: e.tensor_copy(out=csA[:], in_=padded[:]), reads=["padded"], writes=["csA"])
        cur, oth, ck, ok_ = csA, csB, "csA", "csB"
        for s in (1, 2, 4, 8, 16):
            P.op("dve", lambda e, cur=cur, oth=oth: e.tensor_copy(out=oth[:], in_=cur[:]), reads=[ck], writes=[ok_])
            P.op("dve", lambda e, cur=cur, oth=oth, s=s: e.tensor_tensor(out=oth[:, s:32], in0=cur[:, s:32], in1=cur[:, 0:32 - s], op=ALU.add),
                 reads=[ck, ok_], writes=[ok_])
            cur, oth, ck, ok_ = oth, cur, ok_, ck
        pend_t, pend_k = cur, ck
        P.op("dve", lambda e: e.tensor_tensor(out=pstart[:], in0=pend_t[:], in1=padded[:], op=ALU.subtract),
             reads=[pend_k, "padded"], writes=["pstart"])
        Erep = M.alloc("Erep", [128, 64], F32)
        pc = M.alloc("pc", [128, 8], F32)
        idxf = M.alloc("idxf", [128, 8 * 64], F32)
        for bq in range(NBLK):
            P.op("dve", lambda e, bq=bq: e.tensor_single_scalar(out=cmpt[:], in_=pend_t[:], scalar=512.0 * bq, op=ALU.is_le),
                 reads=[pend_k], writes=["cmpt"])
            P.op("dve", lambda e, bq=bq: e.reduce_sum(out=Erep[:, bq:bq + 1], in_=cmpt[:], axis=AX.X), reads=["cmpt"], writes=["Erep"])
        P.op("dve", lambda e: e.tensor_single_scalar(out=Erep[:], in_=Erep[:], scalar=31.0, op=ALU.min), reads=["Erep"], writes=["Erep"])
        for l in range(8):
            P.op("dve", lambda e, l=l: e.tensor_scalar(out=pc[:, l:l + 1], in0=thr, scalar1=1.0 / 512.0, scalar2=128.0 * l,
                                                       op0=ALU.mult, op1=ALU.add), reads=["cst"], writes=["pc"])
        for l in range(8):
            P.op("dve", lambda e, l=l: e.tensor_scalar(out=idxf[:, l * 64:(l + 1) * 64], in0=Erep[:], scalar1=1024.0, scalar2=pc[:, l:l + 1],
                                                       op0=ALU.mult, op1=ALU.add), reads=["Erep", "pc"], writes=["idxf"])
        P.op("dve", lambda e: e.tensor_copy(out=idx_gu[:], in_=idxf[:]), reads=["idxf"], writes=["idx_gu"])
        for l in range(4):
            P.op("dve", lambda e, l=l: e.tensor_scalar(out=idxf[:, l * 64:(l + 1) * 64], in0=Erep[:], scalar1=512.0, scalar2=pc[:, l:l + 1],
                                                       op0=ALU.mult, op1=ALU.add), reads=["Erep", "pc", "idx_gu"], writes=["idxf"])
        P.op("dve", lambda e: e.tensor_copy(out=idx_d[:], in_=idxf[:, 0:256]), reads=["idxf"], writes=["idx_d"])
        P.op("dve", lambda e: e.tensor_scalar(out=idxf[:, 0:64], in0=Erep[:], scalar1=128.0, scalar2=pc[:, 0:1],
                                              op0=ALU.mult, op1=ALU.add), reads=["Erep", "pc", "idx_d"], writes=["idxf"])
        P.op("dve", lambda e: e.tensor_copy(out=idx_b[:], in_=idxf[:, 0:64]), reads=["idxf"], writes=["idx_b"])
        P.op("dve", lambda e: e.tensor_copy(out=idx_e[:], in_=Erep[:]), reads=["Erep"], writes=["idx_e"])
        P.dma("sp", lambda e: e.dma_start(out=bexp_d[0:64, :].rearrange("b o -> o b"), in_=idx_e[0:1, :]), reads=["idx_e"], writes=["bexp_d"])
        xscR = Ring([0, 1, 2])
        for T in range(NT):
            tsl = slice(T * 32, (T + 1) * 32)
            t4 = slice(T * 4, (T + 1) * 4)
            P.op("dve", lambda e, tsl=tsl: e.tensor_tensor(out=dst[:], in0=rank_all[:, tsl], in1=pstart[:], op=ALU.add),
                 reads=["rank", "pstart"], writes=["dst"])
            P.op("dve", lambda e, tsl=tsl: e.scalar_tensor_tensor(out=dst[:], in0=dst[:], scalar=1.0, in1=sel_all[:, tsl],
                                                                  op0=ALU.add, op1=ALU.mult), reads=["dst", "sel"], writes=["dst"])
            P.op("dve", lambda e: e.tensor_single_scalar(out=dst[:], in_=dst[:], scalar=-1.0, op=ALU.add), reads=["dst"], writes=["dst"])
            P.op("dve", lambda e: e.max(out=m8d[:], in_=dst[:]), reads=["dst"], writes=["m8d"])
            P.op("dve", lambda e, t4=t4: e.tensor_copy(out=dest4i[:, t4], in_=m8d[:, 0:4]), reads=["m8d"], writes=["dest4i"])
            P.op("dve", lambda e, tsl=tsl: e.tensor_tensor(out=key2[:], in0=sel_all[:, tsl], in1=iota1, op=ALU.mult),
                 reads=["sel", "cst"], writes=["key2"])
            P.op("dve", lambda e: e.max(out=m8e[:], in_=key2[:]), reads=["key2"], writes=["m8e"])
            for k in range(4):
                P.op("dve", lambda e, k=k, tsl=tsl: e.scalar_tensor_tensor(out=tq[:], in0=key2[:], scalar=m8e[:, k:k + 1],
                                                                           in1=gate_all[:, tsl], op0=ALU.is_equal, op1=ALU.mult),
                     reads=["key2", "m8e", "gate"], writes=["tq"])
                P.op("dve", lambda e, k=k, T=T: e.reduce_sum(out=gate4[:, T * 4 + k: T * 4 + k + 1], in_=tq[:], axis=AX.X),
                     reads=["tq"], writes=["gate4"])
            xi = xscR.next()
            P.dma("sp", lambda e, T=T, xi=xi: e.dma_start(out=xsc[xi][:], in_=x1b_d[T * 128:(T + 1) * 128, :]),
                  reads=["x1b_d"], writes=["xsc%d" % xi])
            for k in range(4):
                P.dma("pool", lambda e, T=T, k=k, xi=xi: e.indirect_dma_start(
                    out=xs_d, out_offset=bass.IndirectOffsetOnAxis(ap=dest4i[:, T * 4 + k: T * 4 + k + 1], axis=0),
                    in_=xsc[xi][:], in_offset=None, bounds_check=get_bc(e), oob_is_err=False),
                    reads=["xsc%d" % xi, "dest4i"], writes=["xs_d"])
        if "rt_s" in dbg:
            P.dma("sp", lambda e: e.dma_start(out=rt_d[:, 0:NT * 4], in_=gate4[:]), reads=["gate4"], writes=["rt_d"])
            P.dma("sp", lambda e: e.dma_start(out=rt_d[:, NT * 4:NT * 8].bitcast(I32), in_=dest4i[:]), reads=["dest4i"], writes=["rt_d"])
        P.barrier()
        M.reset()

    if upto >= 4:
        wsl = [M.alloc("wsl%d" % i, [128, 16 * 512], BF16) for i in range(4)]
        xsb = [M.alloc("xsb%d" % i, [128, 4 * D], BF16) for i in range(2)]
        xbT = M.alloc("xbT", [128, 16 * 512], BF16)
        hT = M.alloc("hT", [128, 16 * 512], BF16)
        gc = [M.alloc("gc%d" % i, [128, 512], F32) for i in range(2)]
        sg = [M.alloc("sg%d" % i, [128, 512], F32) for i in range(2)]
        uc = [M.alloc("uc%d" % i, [128, 512], F32) for i in range(2)]
        yst = [M.alloc("yst%d" % i, [128, 512], F32) for i in range(4)]
        bgu = [M.alloc("bgu%d" % i, [128, 32], F32) for i in range(2)]
        bdr = [M.alloc("bdr%d" % i, [2, D], BF16) for i in range(2)]

        psG = Ring([0, 1])
        psU = Ring([2, 3])
        psY = Ring([4, 5])
        ystR = Ring([0, 1, 2, 3])
        tR = Ring([0, 1])
        wl = [(blk, l) for blk in range(NBLK) for l in range(12)]
        nw = [0]

        def issue_w(upto_idx):
            while nw[0] <= upto_idx and nw[0] < len(wl):
                blk, l = wl[nw[0]]
                i = nw[0] % 4
                if l == 0:
                    bb = blk % 2
                    P.dma("pool", lambda e, blk=blk, bb=bb: e.indirect_dma_start(
                        out=bgu[bb][:, :], out_offset=None, in_=bgu_d,
                        in_offset=bass.IndirectOffsetOnAxis(ap=idx_b[:, blk:blk + 1], axis=0),
                        bounds_check=get_bc(e), oob_is_err=False), reads=["idx"], writes=["bgu%d" % bb])
                    P.dma("pool", lambda e, blk=blk, bb=bb: e.indirect_dma_start(
                        out=bdr[bb][0:2, :], out_offset=None, in_=bd_d,
                        in_offset=bass.IndirectOffsetOnAxis(ap=idx_e[0:2, blk:blk + 1], axis=0),
                        bounds_check=get_bc(e), oob_is_err=False), reads=["idx"], writes=["bdr%d" % bb])

                def ld_w(e, blk=blk, l=l, i=i):
                    if l < 8:
                        src, ia = wgu_d, idx_gu[:, l * 64 + blk: l * 64 + blk + 1]
                    else:
                        src, ia = wd_d, idx_d[:, (l - 8) * 64 + blk: (l - 8) * 64 + blk + 1]
                    return e.indirect_dma_start(out=wsl[i][:, :], out_offset=None, in_=src,
                                                in_offset=bass.IndirectOffsetOnAxis(ap=ia, axis=0),
                                                bounds_check=get_bc(e), oob_is_err=False)
                P.dma("pool", ld_w, reads=["idx"], writes=["wsl%d" % i])
                nw[0] += 1

        def load_xs(blk):
            bb = blk % 2
            P.dma("sp", lambda e: e.dma_start(out=xsb[bb][:].rearrange("p (j d) -> p j d", j=4),
                                              in_=xs_d[blk * 512:(blk + 1) * 512, :].rearrange("(j p) d -> p j d", p=128)),
                  reads=["xs_d"], writes=["xsb%d" % bb])

        load_xs(0)
        ecount = [0]
        for blk in range(NBLK):
            bb = blk % 2
            issue_w(blk * 12 + 2)
            if blk + 1 < NBLK:
                load_xs(blk + 1)
            for kc in range(16):
                hb = kc % 2
                for j in range(4):
                    P.op("pe", lambda e, kc=kc, j=j, hb=hb, bb=bb: e.transpose(
                        out=psbs[hb][:, j * 128:(j + 1) * 128],
                        in_=xsb[bb][:, j * D + kc * 128: j * D + (kc + 1) * 128], identity=identb[:]),
                        reads=["xsb%d" % bb, "identb"], writes=["psb%d" % hb])
                copy_op(evac(kc), xbT[:, kc * 512:(kc + 1) * 512], psbs[hb][:, 0:512], ["psb%d" % hb], ["xbT"])
            for l in range(12):
                wi_idx = blk * 12 + l
                issue_w(wi_idx + 3)
                wi = wi_idx % 4
                wt = wsl[wi]
                wkey = "wsl%d" % wi
                if l < 8:
                    for s2 in range(2):
                        fci = 2 * l + s2
                        pg = psG.next()
                        pu = psU.next()
                        for kc in range(16):
                            P.op("pe", lambda e, pg=pg, kc=kc, s2=s2, wt=wt: e.matmul(
                                psf[pg][:, 0:512], lhsT=wt[:, kc * 512 + s2 * 128: kc * 512 + (s2 + 1) * 128],
                                rhs=xbT[:, kc * 512:(kc + 1) * 512], start=(kc == 0), stop=(kc == 15)),
                                reads=[wkey, "xbT"], writes=["psf%d" % pg])
                        for kc in range(16):
                            P.op("pe", lambda e, pu=pu, kc=kc, s2=s2, wt=wt: e.matmul(
                                psf[pu][:, 0:512], lhsT=wt[:, kc * 512 + 256 + s2 * 128: kc * 512 + 256 + (s2 + 1) * 128],
                                rhs=xbT[:, kc * 512:(kc + 1) * 512], start=(kc == 0), stop=(kc == 15)),
                                reads=[wkey, "xbT"], writes=["psf%d" % pu])
                        ti = tR.next()
                        P.op("dve", lambda e, pg=pg, ti=ti, fci=fci, bb=bb: e.tensor_scalar(
                            out=gc[ti][:], in0=psf[pg][:, 0:512], scalar1=bgu[bb][:, fci:fci + 1], scalar2=7.0,
                            op0=ALU.add, op1=ALU.min), reads=["psf%d" % pg, "bgu%d" % bb], writes=["gc%d" % ti])
                        P.op("act", lambda e, ti=ti: e.activation(out=sg[ti][:], in_=gc[ti][:], func=AF.Sigmoid, scale=1.702),
                             reads=["gc%d" % ti], writes=["sg%d" % ti])
                        P.op("dve", lambda e, pu=pu, ti=ti, fci=fci, bb=bb: e.tensor_scalar(
                            out=uc[ti][:], in0=psf[pu][:, 0:512], scalar1=bgu[bb][:, 16 + fci:17 + fci], scalar2=7.0,
                            op0=ALU.add, op1=ALU.min), reads=["psf%d" % pu, "bgu%d" % bb], writes=["uc%d" % ti])
                        P.op("dve", lambda e, ti=ti: e.tensor_scalar(
                            out=uc[ti][:], in0=uc[ti][:], scalar1=-7.0, scalar2=1.0, op0=ALU.max, op1=ALU.add),
                            reads=["uc%d" % ti], writes=["uc%d" % ti])
                        P.op("dve", lambda e, ti=ti: e.tensor_tensor(out=uc[ti][:], in0=uc[ti][:], in1=gc[ti][:], op=ALU.mult),
                             reads=["uc%d" % ti, "gc%d" % ti], writes=["uc%d" % ti])
                        P.op("dve", lambda e, ti=ti, fci=fci: e.tensor_tensor(
                            out=hT[:, fci * 512:(fci + 1) * 512], in0=uc[ti][:], in1=sg[ti][:], op=ALU.mult),
                            reads=["uc%d" % ti, "sg%d" % ti], writes=["hT"])
                else:
                    lc = l - 8
                    for j in range(4):
                        py = psY.next()
                        for fc in range(16):
                            P.op("pe", lambda e, py=py, fc=fc, j=j, wt=wt: e.matmul(
                                psf[py][:, 0:512], lhsT=hT[:, fc * 512 + j * 128: fc * 512 + (j + 1) * 128],
                                rhs=wt[:, fc * 512:(fc + 1) * 512], start=(fc == 0), stop=False),
                                reads=[wkey, "hT"], writes=["psf%d" % py])
                        P.op("pe", lambda e, py=py, lc=lc, bb=bb: e.matmul(
                            psf[py][:, 0:512], lhsT=onesrow[0:1, 0:128], rhs=bdr[bb][0:1, lc * 512:(lc + 1) * 512],
                            start=False, stop=True), reads=["onesrow", "bdr%d" % bb], writes=["psf%d" % py])
                        yi = ystR.next()
                        ecount[0] += 1
                        copy_op(evac(ecount[0]), yst[yi][:], psf[py][:, 0:512], ["psf%d" % py], ["yst%d" % yi])
                        P.dma("sp", lambda e, blk=blk, j=j, lc=lc, yi=yi: e.dma_start(
                            out=ys_d[blk * 512 + j * 128: blk * 512 + (j + 1) * 128, lc * 512:(lc + 1) * 512], in_=yst[yi][:]),
                            reads=["yst%d" % yi], writes=["ys_d"])
        P.barrier()
        M.reset()

    if upto >= 5:
        lng = M.alloc("lng6", [128, D], F32)
        lnb = M.alloc("lnb6", [128, D], F32)
        yg = [M.alloc("yg%d" % i, [128, 4 * D], F32) for i in range(2)]
        x1l = [M.alloc("x1l%d" % i, [128, D], F32) for i in range(2)]
        acc = [M.alloc("acc%d" % i, [128, D], F32) for i in range(2)]
        ot = [M.alloc("ot%d" % i, [128, D], F32) for i in range(2)]
        stats = M.alloc("stats6", [128, 24], F32)
        mv = M.alloc("mv6", [128, 2], F32)
        tmp2 = M.alloc("tmp26", [128, 2], F32)
        P.dma("sp", lambda e, t=lng: e.dma_start(out=t[:], in_=ln2g_d.partition_broadcast(128)), writes=["lnconst_a"])
        P.dma("sp", lambda e, t=lnb: e.dma_start(out=t[:], in_=ln2b_d.partition_broadcast(128)), writes=["lnconst_b"])

        def gather(T):
            b = T % 2
            for k in range(4):
                P.dma("pool", lambda e, k=k: e.indirect_dma_start(
                    out=yg[b][:, k * D:(k + 1) * D], out_offset=None, in_=ys_d,
                    in_offset=bass.IndirectOffsetOnAxis(ap=dest4i[:, T * 4 + k: T * 4 + k + 1], axis=0),
                    bounds_check=get_bc(e), oob_is_err=False), reads=["ys_d", "dest4i"], writes=["yg%d_%d" % (b, k)])
            P.dma("sp", lambda e: e.dma_start(out=x1l[b][:], in_=x1_d[T * 128:(T + 1) * 128, :]), reads=["x1_d"], writes=["x1l%d" % b])

        gather(0)
        for T in range(NT):
            b = T % 2
            if T + 1 < NT:
                gather(T + 1)
            ak = "acc%d" % b
            P.op("act", lambda e, b=b: e.mul(acc[b][:], x1l[b][:], DN_ALPHA),
                 reads=["x1l%d" % b], writes=[ak])
            for k in range(4):
                P.op("dve", lambda e, b=b, k=k, T=T: e.scalar_tensor_tensor(
                    out=acc[b][:], in0=yg[b][:, k * D:(k + 1) * D], scalar=gate4[:, T * 4 + k: T * 4 + k + 1], in1=acc[b][:],
                    op0=ALU.mult, op1=ALU.add), reads=["yg%d_%d" % (b, k), "gate4", ak], writes=[ak])
            layer_norm_rows(acc[b][:], [ak], lng[:], lnb[:], ot[b][:], "ot%d" % b, stats, mv, tmp2, D, "l2_")
            P.dma("sp", lambda e, T=T, b=b: e.dma_start(out=out_d[T * 128:(T + 1) * 128, :], in_=ot[b][:]),
                  reads=["ot%d" % b], writes=["out_d"])
    P.emit()
    return nc


def _mask_const():
    kj = np.arange(128)[:, None, None]
    i = np.arange(17)[None, :, None]
    qi = np.arange(128)[None, None, :]
    o = 16 - i
    dl = 128 * o + qi - kj
    m = ((dl >= 0) & (dl <= 128)).astype(np.float32)
    m += ((dl >= 0) & (dl <= 512) & (dl % 4 == 0)).astype(np.float32)
    m += ((dl >= 0) & (dl <= 2048) & (dl % 16 == 0)).astype(np.float32)
    return np.ascontiguousarray(m.reshape(128, 17 * 128))


def _consts():
    c = np.zeros((128, 417), np.float32)
    c[:, 0:128] = np.eye(128, dtype=np.float32)
    tp = np.arange(128)[:, None]
    t = np.arange(128)[None, :]
    c[:, 128:256] = (tp < t).astype(np.float32)
    c[:, 256:288] = np.arange(1, 33, dtype=np.float32)[None, :]
    c[:, 288] = np.arange(128, dtype=np.float32) * 512.0
    c[:, 289:417] = 1.0
    return c


def prep_shared(inp):
    f = lambda a: np.ascontiguousarray(np.asarray(a, dtype=np.float32))
    w_in = f(inp["w_in"])[0]
    w_in_p = np.zeros((D, 4608), np.float32)
    w_in_p[:, :4352] = w_in
    sh = {}
    sh["w_in_r"] = np.ascontiguousarray(w_in_p.reshape(16, 128, 9, 512).transpose(2, 1, 0, 3)).reshape(9, 128, 16 * 512)
    sh["w_spatial"] = f(inp["w_spatial"])[0]
    sh["b_spatial"] = f(inp["b_spatial"])[0].reshape(1, 768)
    sh["a_ln_g"] = f(inp["a_ln_g"]).reshape(1, 768)
    sh["a_ln_b"] = f(inp["a_ln_b"]).reshape(1, 768)
    sh["w_kv_r"] = np.ascontiguousarray(f(inp["w_mem_kv"])[0].reshape(16, 128, 2, 512).transpose(2, 1, 0, 3)).reshape(2, 128, 16 * 512)
    sh["norm_a_g"] = f(inp["norm_a_g"]).reshape(768)
    sh["norm_b_g"] = f(inp["norm_b_g"]).reshape(768)
    sh["norm_m_g"] = f(inp["norm_m_g"]).reshape(512)
    sh["w_out_r"] = np.ascontiguousarray(f(inp["w_out"])[0].reshape(16, 128, D).transpose(1, 0, 2)).reshape(128, 16 * D)
    sh["ln1_g"] = f(inp["ln1_g"]).reshape(1, D)
    sh["ln1_b"] = f(inp["ln1_b"]).reshape(1, D)
    sh["w_router_r"] = np.ascontiguousarray(f(inp["w_router"])[0].reshape(16, 128, 32).transpose(1, 0, 2)).reshape(128, 16 * 32)
    sh["b_router"] = f(inp["b_router"]).reshape(1, 32)
    wgu = f(inp["w_gate_up"])[0]
    NEP = wgu.shape[0]
    v = wgu.reshape(NEP, 16, 128, 2, 8, 256)
    sh["w_gu_r"] = np.ascontiguousarray(v.transpose(0, 4, 2, 1, 3, 5)).reshape(NEP * 8 * 128, 16 * 512)
    sh["b_gu_r"] = np.ascontiguousarray(f(inp["b_gate_up"])[0].reshape(NE, 32, 128).transpose(0, 2, 1)).reshape(NE * 128, 32)
    wd = f(inp["w_down"])[0]
    v = wd.reshape(NEP, 16, 128, 4, 512)
    sh["w_d_r"] = np.ascontiguousarray(v.transpose(0, 3, 2, 1, 4)).reshape(NEP * 4 * 128, 16 * 512)
    sh["b_down"] = f(inp["b_down"])[0].reshape(NE, D)
    sh["ln2_g"] = f(inp["ln2_g"]).reshape(1, D)
    sh["ln2_b"] = f(inp["ln2_b"]).reshape(1, D)
    sh["cst"] = _consts()
    sh["maskc"] = _mask_const()
    return sh


def kernel(**inputs):
    x = np.asarray(inputs["x"], dtype=np.float32)
    mem = np.asarray(inputs["mem"], dtype=np.float32)
    sh = prep_shared(inputs)
    nc = build()
    in_maps = []
    for c in range(8):
        m = dict(sh)
        m["x"] = np.ascontiguousarray(x[c])
        m["mem"] = np.ascontiguousarray(mem[c])
        in_maps.append(m)
    res = run_bass_kernel_spmd(nc, in_maps, core_ids=list(range(8)))
    return np.stack([np.asarray(r["out"], dtype=np.float32) for r in res.results], axis=0)
```

```python
import numpy as np
import concourse.bass as bass
import concourse.mybir as mybir
from concourse.bass_utils import run_bass_kernel_spmd

F32 = mybir.dt.float32
BF16 = mybir.dt.bfloat16
I32 = mybir.dt.int32
AF = mybir.ActivationFunctionType
ALU = mybir.AluOpType
AX = mybir.AxisListType

ENGS = ["pe", "act", "dve", "pool", "sp"]
SEM_ROT = 12000
NDS = 6

S = 4096
D = 2048
NT = S // 128
GT = 512
NG = S // GT
NE = 32
NBLK = 64
DN_ALPHA = 2.0 ** 0.25
LN_EPS = 1e-5
SCALE = 128.0 ** -0.5


class Prog:
    def __init__(self, nc, same_engine_sync=True):
        self.nc = nc
        self.ops = {e: [] for e in ENGS}
        self.cnt = {e: 0 for e in ENGS}
        self.dcnt = {e: 0 for e in ENGS}
        self.lastw = {}
        self.readers = {}
        self.waited = {e: {} for e in ENGS}
        self.pend = {e: {} for e in ENGS}
        self.sems = {}
        self.same = same_engine_sync
        self.final = {}

    def _deps(self, eng, reads, writes, nosame):
        deps = set()
        for r in reads:
            t = self.lastw.get(r)
            if t is not None:
                deps.add(t)
        for w in writes:
            t = self.lastw.get(w)
            if t is not None:
                deps.add(t)
            for t in self.readers.get(w, ()):
                deps.add(t)
        out = dict(self.pend[eng])
        self.pend[eng] = {}
        for (s, v, e) in deps:
            if e == eng and (eng == "pe" or nosame or not self.same):
                continue
            out[s] = max(out.get(s, 0), v)
        res = []
        for s, v in out.items():
            if self.waited[eng].get(s, 0) >= v:
                continue
            self.waited[eng][s] = v
            res.append((s, v))
        return res

    def _commit(self, tok, reads, writes):
        for r in reads:
            self.readers.setdefault(r, []).append(tok)
        for w in writes:
            self.lastw[w] = tok
            self.readers[w] = []

    def op(self, eng, fn, reads=(), writes=(), nosame=False):
        waits = self._deps(eng, reads, writes, nosame)
        i = self.cnt[eng]
        self.cnt[eng] += 1
        s = "c_%s_%d" % (eng, i // SEM_ROT)
        tok = (s, i % SEM_ROT + 1, eng)
        self.ops[eng].append((waits, fn, s, 1))
        self._commit(tok, reads, writes)
        return tok

    def dma(self, eng, fn, reads=(), writes=()):
        waits = dict(self._deps(eng, reads, writes, False))
        j = self.dcnt[eng]
        self.dcnt[eng] += 1
        s = "d_%s_%d" % (eng, j % NDS)
        v = 16 * (j // NDS + 1)
        if v > 16 and self.waited[eng].get(s, 0) < v - 16:
            waits[s] = max(waits.get(s, 0), v - 16)
            self.waited[eng][s] = v - 16
        tok = (s, v, "dma_" + eng)
        self.ops[eng].append((list(waits.items()), fn, s, 16))
        self._commit(tok, reads, writes)
        self.final[s] = v
        return tok

    def _all_tokens(self):
        toks = []
        for e in ENGS:
            if self.cnt[e] > 0:
                i = self.cnt[e] - 1
                toks.append(("c_%s_%d" % (e, i // SEM_ROT), i % SEM_ROT + 1))
            j = self.dcnt[e]
            for jj in range(max(0, j - NDS), j):
                toks.append(("d_%s_%d" % (e, jj % NDS), 16 * (jj // NDS + 1)))
        return toks

    def barrier(self):
        toks = self._all_tokens()
        for e in ENGS:
            for (s, v) in toks:
                if self.waited[e].get(s, 0) >= v:
                    continue
                self.pend[e][s] = max(self.pend[e].get(s, 0), v)
        self.lastw = {}
        self.readers = {}

    def emit(self):
        nc = self.nc
        names = set()
        for e in ENGS:
            for (waits, fn, s, n) in self.ops[e]:
                names.add(s)
                for (ws, wv) in waits:
                    names.add(ws)
        for nm in sorted(names):
            self.sems[nm] = nc.alloc_semaphore(name=nm)
        fin = dict(self._all_tokens())
        for s, v in self.final.items():
            fin[s] = max(fin.get(s, 0), v)
        with nc.Block() as block:
            def body(ename):
                def run(eng):
                    for (waits, fn, s, n) in self.ops[ename]:
                        for (ws, wv) in waits:
                            eng.wait_ge(self.sems[ws], wv)
                        fn(eng).then_inc(self.sems[s], n)
                    if ename == "sp":
                        for s, v in fin.items():
                            eng.wait_ge(self.sems[s], v)
                return run
            block.tensor(body("pe"))
            block.scalar(body("act"))
            block.vector(body("dve"))
            block.gpsimd(body("pool"))
            block.sync(body("sp"))


class Mem:
    BASE = 16512
    TOP = 229344

    def __init__(self, nc):
        self.nc = nc
        self.ptr = self.BASE
        self.persist = self.BASE
        self.n = 0

    def alloc(self, name, shape, dtype):
        esz = 2 if dtype == BF16 else 4
        size = esz
        for d in shape[1:]:
            size *= d
        size = (size + 31) // 32 * 32
        off = self.ptr
        self.ptr += size
        assert self.ptr <= self.TOP, ("SBUF overflow", name, self.ptr)
        self.n += 1
        return self.nc.alloc_sbuf_tensor_at("%s_%d" % (name, self.n), list(shape), dtype, offset=off)

    def keep(self):
        self.persist = self.ptr

    def reset(self):
        self.ptr = self.persist


class Ring:
    def __init__(self, items):
        self.items = items
        self.i = 0

    def next(self):
        it = self.items[self.i % len(self.items)]
        self.i += 1
        return it


PARTS = [("u", 0, 768, "f"), ("v", 768, 768, "t"), ("q", 1536, 768, "f"), ("k", 2304, 768, "f"),
         ("va", 3072, 768, "t"), ("m", 3840, 512, "f")]


def build(upto=99, dbg=()):
    nc = bass.Bass("TRN2", target_bir_lowering=False)
    P = Prog(nc)
    M = Mem(nc)

    def dram_in(name, shape, dt=F32):
        return nc.dram_tensor(name, list(shape), dt, kind="ExternalInput").ap()

    def dram_scr(name, shape, dt):
        kind = "ExternalOutput" if name in dbg else "Internal"
        return nc.dram_tensor(name, list(shape), dt, kind=kind).ap()

    x_d = dram_in("x", [S, D])
    mem_d = dram_in("mem", [256, D])
    win_d = dram_in("w_in_r", [9, 128, 16 * 512])
    wsp_d = dram_in("w_spatial", [6, 128, 128])
    bsp_d = dram_in("b_spatial", [1, 768])
    alng_d = dram_in("a_ln_g", [1, 768])
    alnb_d = dram_in("a_ln_b", [1, 768])
    wkv_d = dram_in("w_kv_r", [2, 128, 16 * 512])
    ga_d = dram_in("norm_a_g", [768])
    gb_d = dram_in("norm_b_g", [768])
    gm_d = dram_in("norm_m_g", [512])
    wout_d = dram_in("w_out_r", [128, 16 * 2048])
    ln1g_d = dram_in("ln1_g", [1, D])
    ln1b_d = dram_in("ln1_b", [1, D])
    wr_d = dram_in("w_router_r", [128, 16 * 32])
    br_d = dram_in("b_router", [1, 32])
    import os as _os2
    NEW = NE if (upto >= 4 and not _os2.environ.get("FORCE_NEW1")) else 1
    wgu_d = dram_in("w_gu_r", [NEW * 8 * 128, 16 * 512])
    bgu_d = dram_in("b_gu_r", [NE * 128, 32])
    wd_d = dram_in("w_d_r", [NEW * 4 * 128, 16 * 512])
    bd_d = dram_in("b_down", [NE, D])
    ln2g_d = dram_in("ln2_g", [1, D])
    ln2b_d = dram_in("ln2_b", [1, D])
    cst_d = dram_in("cst", [128, 417])
    mask_d = dram_in("maskc", [128, 17 * 128])
    out_d = nc.dram_tensor("out", [S, D], F32, kind="ExternalOutput").ap()

    qT_d = dram_scr("qT_s", [6, 128, S], BF16)
    kT_d = dram_scr("kT_s", [6, 128, S], BF16)
    v_d = dram_scr("v_s", [S, 768], BF16)
    qmT_d = dram_scr("qmT_s", [4, 128, S], BF16)
    yT_d = dram_scr("yT_s", [16, 128, S], BF16)
    x1_d = dram_scr("x1_s", [S, D], F32)
    x1b_d = dram_scr("x1b_s", [S, D], BF16)
    xs_d = dram_scr("xs_s", [NBLK * 512, D], BF16)
    ys_d = dram_scr("ys_s", [NBLK * 512, D], F32)
    bexp_d = dram_scr("bexp_s", [128, 1], I32)
    rt_d = dram_scr("rt_s", [128, NT * 8], F32)

    psf = [nc.alloc_psum_tensor("psf%d" % i, [128, 512], F32) for i in range(6)]
    psbs = [nc.alloc_psum_tensor("psb%d" % i, [128, 1024], BF16) for i in range(2)]

    cst = M.alloc("cst", [128, 417], F32)
    identf = cst[:, 0:128]
    iota1 = cst[:, 256:288]
    thr = cst[:, 288:289]
    identb = M.alloc("identb", [128, 128], BF16)
    trib = M.alloc("trib", [128, 128], BF16)
    onesb = M.alloc("onesb", [128, 128], BF16)
    onesrow = M.alloc("onesrow", [1, 512], BF16)
    onesrow32 = M.alloc("onesrow32", [1, 128], F32)
    KmT = M.alloc("KmT", [128, 4 * 256], BF16)
    Vm = M.alloc("Vm", [128, 2 * 512], BF16)
    gA = M.alloc("gA", [128, 6], F32)
    gB = M.alloc("gB", [128, 6], F32)
    gM = M.alloc("gM", [128, 4], F32)
    rank_all = M.alloc("rank_all", [128, NT * 32], F32)
    sel_all = M.alloc("sel_all", [128, NT * 32], F32)
    gate_all = M.alloc("gate_all", [128, NT * 32], F32)
    dest4i = M.alloc("dest4i", [128, NT * 4], I32)
    gate4 = M.alloc("gate4", [128, NT * 4], F32)
    idx_gu = M.alloc("idx_gu", [128, 8 * 64], I32)
    idx_d = M.alloc("idx_d", [128, 4 * 64], I32)
    idx_b = M.alloc("idx_b", [128, 64], I32)
    idx_e = M.alloc("idx_e", [128, 64], I32)
    M.keep()

    P.dma("sp", lambda e: e.dma_start(out=cst[:], in_=cst_d), writes=["cst"])
    P.dma("sp", lambda e: e.dma_start(out=gA[:], in_=ga_d.rearrange("(h p) -> p h", p=128), allow_slow_non_contiguous=True), writes=["gA"])
    P.dma("sp", lambda e: e.dma_start(out=gB[:], in_=gb_d.rearrange("(h p) -> p h", p=128), allow_slow_non_contiguous=True), writes=["gB"])
    P.dma("sp", lambda e: e.dma_start(out=gM[:], in_=gm_d.rearrange("(h p) -> p h", p=128), allow_slow_non_contiguous=True), writes=["gM"])
    P.op("dve", lambda e: e.tensor_copy(out=identb[:], in_=cst[:, 0:128]), reads=["cst"], writes=["identb"])
    P.op("dve", lambda e: e.tensor_copy(out=trib[:], in_=cst[:, 128:256]), reads=["cst"], writes=["trib"])
    P.op("dve", lambda e: e.tensor_copy(out=onesb[:], in_=cst[:, 289:417]), reads=["cst"], writes=["onesb"])
    P.op("dve", lambda e: e.memset(onesrow[:], 1.0), writes=["onesrow"])
    P.op("dve", lambda e: e.memset(onesrow32[:], 1.0), writes=["onesrow32"])

    import os as _os
    _EV = _os.environ.get("EVAC", "")

    _bc = {}

    def get_bc(e):
        if "v" not in _bc:
            r = e.alloc_register("bnd")
            e.reg_mov(r, NBLK * 512 - 1)
            _bc["v"] = e.snap(r)
        return _bc["v"]

    def evac(i, **kw):
        if _EV:
            return _EV
        return "act" if i % 2 == 0 else "dve"

    def copy_op(eng, out, in_, reads, writes):
        if eng == "act":
            P.op("act", lambda e: e.activation(out=out, in_=in_, func=AF.Copy), reads=reads, writes=writes)
        else:
            P.op(eng, lambda e: e.tensor_copy(out=out, in_=in_), reads=reads, writes=writes)

    def rstd_rep(pss_key, pss_ap, nfeat, tmp, rrep, key):
        P.op("act", lambda e: e.activation(out=tmp, in_=pss_ap, func=AF.Sqrt, bias=LN_EPS, scale=1.0 / nfeat),
             reads=[pss_key], writes=[key + "_t"])
        P.op("dve", lambda e: e.reciprocal(out=rrep, in_=tmp), reads=[key + "_t"], writes=[key])

    def layer_norm_rows(h, hkeys, gb_t, bb_t, out, outkey, stats, mv, tmp2, width, pfx):
        nch = width // 512 if width % 512 == 0 else 2
        cw = width // nch
        for c in range(nch):
            P.op("dve", lambda e, c=c: e.bn_stats(out=stats[:, c * 6:(c + 1) * 6], in_=h[:, c * cw:(c + 1) * cw]),
                 reads=hkeys, writes=[pfx + "st%d" % c])
        P.op("dve", lambda e: e.bn_aggr(out=mv[:, 0:2], in_=stats[:, 0:nch * 6]),
             reads=[pfx + "st%d" % c for c in range(nch)], writes=[pfx + "mv"])
        P.op("act", lambda e: e.activation(out=tmp2[:, 0:1], in_=mv[:, 1:2], func=AF.Sqrt, bias=LN_EPS, scale=1.0),
             reads=[pfx + "mv"], writes=[pfx + "sd"])
        P.op("dve", lambda e: e.reciprocal(out=tmp2[:, 1:2], in_=tmp2[:, 0:1]), reads=[pfx + "sd"], writes=[pfx + "rs"])
        P.op("dve", lambda e: e.scalar_tensor_tensor(out=h, in0=h, scalar=mv[:, 0:1], in1=gb_t, op0=ALU.subtract, op1=ALU.mult),
             reads=list(hkeys) + [pfx + "mv", "lnconst_a"], writes=list(hkeys))
        P.op("dve", lambda e: e.scalar_tensor_tensor(out=out, in0=h, scalar=tmp2[:, 1:2], in1=bb_t, op0=ALU.mult, op1=ALU.add),
             reads=list(hkeys) + [pfx + "rs", "lnconst_b"], writes=[outkey])

    if upto >= 1:
        WmT = M.alloc("WmT", [128, 6 * 128], BF16)
        bsp = M.alloc("bsp", [1, 768], BF16)
        alng = M.alloc("alng", [128, 768], F32)
        alnb = M.alloc("alnb", [128, 768], F32)
        wspf = M.alloc("wspf", [128, 6 * 128], F32)
        wspb = M.alloc("wspb", [128, 6 * 128], BF16)
        xb = [M.alloc("xb%d" % i, [128, 4 * D], BF16) for i in range(2)]
        xT = M.alloc("xT", [128, 16 * 512], BF16)
        wch = [M.alloc("wch%d" % i, [128, 16 * 512], BF16) for i in range(3)]
        uT = M.alloc("uT", [128, 6 * 512], BF16)
        vf = M.alloc("vf", [128, 4 * 768], F32)
        vn = M.alloc("vn", [128, 4 * 768], BF16)
        vast = M.alloc("vast", [128, 4 * 768], BF16)
        fst = [M.alloc("fst%d" % i, [128, 512], BF16) for i in range(4)]
        yaT = M.alloc("yaT", [128, 6 * 512], BF16)
        sqA = M.alloc("sqA", [128, 6 * 512], BF16)
        yan = M.alloc("yan", [128, 6 * 512], BF16)
        rtmp = M.alloc("rtmp", [128, 512], F32)
        rrep = M.alloc("rrep", [128, 512], F32)
        stats = M.alloc("stats", [128, 24], F32)
        mv = M.alloc("mv", [128, 2], F32)
        tmp2 = M.alloc("tmp2", [128, 2], F32)

        P.dma("sp", lambda e: e.dma_start(out=wspf[:].rearrange("p (g s) -> p g s", g=6), in_=wsp_d.rearrange("g t s -> t g s")), writes=["wspf"])
        P.dma("pool", lambda e: e.dma_start(out=bsp[:], in_=bsp_d), writes=["bsp"])
        P.dma("sp", lambda e: e.dma_start(out=alng[:], in_=alng_d.partition_broadcast(128)), writes=["lnconst_a"])
        P.dma("sp", lambda e: e.dma_start(out=alnb[:], in_=alnb_d.partition_broadcast(128)), writes=["lnconst_b"])
        for g in range(6):
            P.op("pool", lambda e, g=g: e.affine_select(out=wspf[:, g * 128:(g + 1) * 128], in_=wspf[:, g * 128:(g + 1) * 128],
                                                        pattern=[[-1, 128]], compare_op=ALU.is_ge, fill=0.0, base=0,
                                                        channel_multiplier=1), reads=["wspf"], writes=["wspf"])
        P.op("dve", lambda e: e.tensor_copy(out=wspb[:], in_=wspf[:]), reads=["wspf"], writes=["wspb"])
        for g in range(6):
            P.op("pe", lambda e, g=g: e.transpose(out=psbs[0][:, g * 128:(g + 1) * 128], in_=wspb[:, g * 128:(g + 1) * 128], identity=identb[:]),
                 reads=["wspb", "identb"], writes=["psb0"])
        P.op("dve", lambda e: e.tensor_copy(out=WmT[:], in_=psbs[0][:, 0:768]), reads=["psb0"], writes=["WmT"])

        psA = Ring([0, 1, 2])
        psM = Ring([3, 4])
        fstR = Ring([0, 1, 2, 3])
        loads = [(G, c) for G in range(NG) for c in range(9)]
        nload = [0]
        ev = [0]

        def issue_loads(upto_idx):
            while nload[0] <= upto_idx and nload[0] < len(loads):
                G, c = loads[nload[0]]
                if c == 0:
                    b = G % 2
                    for jx in range(4):
                        P.dma("pool", lambda e, G=G, b=b, jx=jx: e.dma_start(
                            out=xb[b][:, jx * D:(jx + 1) * D],
                            in_=x_d[G * GT + jx * 128: G * GT + (jx + 1) * 128, :]), writes=["xb%d_%d" % (b, jx)])
                i = nload[0] % 3
                P.dma("pool", lambda e, c=c, i=i: e.dma_start(out=wch[i][:], in_=win_d[c]), writes=["wch%d" % i])
                nload[0] += 1

        import os
        P1_NG = int(os.environ.get("P1_NG", NG))
        P1_NC = int(os.environ.get("P1_NC", 9))
        P1_TR = int(os.environ.get("P1_TR", 16))
        for G in range(P1_NG):
            gsl = slice(G * GT, (G + 1) * GT)
            b = G % 2
            issue_loads(G * 9 + 1)
            for kc in range(P1_TR):
                hb = kc % 2
                for j in range(4):
                    P.op("pe", lambda e, kc=kc, j=j, hb=hb, b=b: e.transpose(
                        out=psbs[hb][:, j * 128:(j + 1) * 128],
                        in_=xb[b][:, j * D + kc * 128: j * D + (kc + 1) * 128], identity=identb[:]),
                        reads=["xb%d_%d" % (b, j), "identb"], writes=["psb%d" % hb])
                copy_op(evac(kc), xT[:, kc * 512:(kc + 1) * 512], psbs[hb][:, 0:512], ["psb%d" % hb], ["xT"])
            for c in range(P1_NC):
                li = G * 9 + c
                issue_loads(li + 2)
                wi = li % 3
                wt = wch[wi]
                wkey = "wch%d" % wi
                c0 = c * 512
                for (pname, pstart, pwidth, lay) in PARTS:
                    lo = max(pstart, c0)
                    hi = min(pstart + pwidth, c0 + 512)
                    if lo >= hi:
                        continue
                    if lay == "f":
                        for blk in range((lo - pstart) // 128, (hi - pstart) // 128):
                            off = pstart + blk * 128 - c0
                            pi = psA.next()
                            for kc in range(16):
                                P.op("pe", lambda e, pi=pi, kc=kc, off=off, wt=wt: e.matmul(
                                    psf[pi][:, 0:512], lhsT=wt[:, kc * 512 + off: kc * 512 + off + 128],
                                    rhs=xT[:, kc * 512:(kc + 1) * 512], start=(kc == 0), stop=(kc == 15)),
                                    reads=[wkey, "xT"], writes=["psf%d" % pi])
                            if pname == "u":
                                P.op("act", lambda e, pi=pi, blk=blk: e.activation(
                                    out=uT[:, blk * 512:(blk + 1) * 512], in_=psf[pi][:, 0:512], func=AF.Gelu_apprx_tanh),
                                    reads=["psf%d" % pi], writes=["uT%d" % blk])
                            else:
                                fi = fstR.next()
                                ev[0] += 1
                                copy_op(evac(ev[0]), fst[fi][:], psf[pi][:, 0:512], ["psf%d" % pi], ["fst%d" % fi])
                                dst = {"q": qT_d, "k": kT_d, "m": qmT_d}[pname]
                                P.dma("sp", lambda e, dst=dst, blk=blk, fi=fi, gsl=gsl: e.dma_start(out=dst[blk, :, gsl], in_=fst[fi][:]),
                                      reads=["fst%d" % fi], writes=[pname + "_d"])
                    else:
                        wd = hi - lo
                        off = lo - c0
                        oc = lo - pstart
                        for j in range(4):
                            pi = psA.next()
                            for kc in range(16):
                                P.op("pe", lambda e, pi=pi, kc=kc, off=off, wd=wd, j=j, wt=wt: e.matmul(
                                    psf[pi][:, 0:wd], lhsT=xT[:, kc * 512 + j * 128: kc * 512 + (j + 1) * 128],
                                    rhs=wt[:, kc * 512 + off: kc * 512 + off + wd], start=(kc == 0), stop=(kc == 15)),
                                    reads=[wkey, "xT"], writes=["psf%d" % pi])
                            if pname == "v":
                                P.op("act", lambda e, pi=pi, j=j, oc=oc, wd=wd: e.activation(
                                    out=vf[:, j * 768 + oc: j * 768 + oc + wd], in_=psf[pi][:, 0:wd], func=AF.Gelu_apprx_tanh),
                                    reads=["psf%d" % pi], writes=["vf%d_%d" % (j, oc)])
                            else:
                                ev[0] += 1
                                copy_op(evac(ev[0]), vast[:, j * 768 + oc: j * 768 + oc + wd], psf[pi][:, 0:wd],
                                        ["psf%d" % pi], ["vast%d_%d" % (j, oc)])
                                if oc + wd == 768:
                                    P.dma("sp", lambda e, j=j, G=G: e.dma_start(
                                        out=v_d[G * GT + j * 128: G * GT + (j + 1) * 128, :], in_=vast[:, j * 768:(j + 1) * 768]),
                                        reads=["vast%d_0" % j, "vast%d_512" % j], writes=["v_d"])
                if c == 2:
                    for j in range(4):
                        hj = vf[:, j * 768:(j + 1) * 768]
                        layer_norm_rows(hj, ["vf%d_0" % j, "vf%d_256" % j], alng[:], alnb[:], vn[:, j * 768:(j + 1) * 768], "vn%d" % j,
                                        stats, mv, tmp2, 768, "g_")
                    for g in range(6):
                        pi = psM.next()
                        for j in range(4):
                            P.op("pe", lambda e, pi=pi, j=j, g=g: e.matmul(
                                psf[pi][:, j * 128:(j + 1) * 128], lhsT=vn[:, j * 768 + g * 128: j * 768 + (g + 1) * 128],
                                rhs=WmT[:, g * 128:(g + 1) * 128], start=True, stop=False),
                                reads=["vn%d" % j, "WmT"], writes=["psf%d" % pi])
                            P.op("pe", lambda e, pi=pi, j=j, g=g: e.matmul(
                                psf[pi][:, j * 128:(j + 1) * 128], lhsT=onesrow[0:1, 0:128],
                                rhs=bsp[0:1, g * 128:(g + 1) * 128], start=False, stop=True),
                                reads=["onesrow", "bsp"], writes=["psf%d" % pi])
                        P.op("dve", lambda e, pi=pi, g=g: e.tensor_tensor(
                            out=yaT[:, g * 512:(g + 1) * 512], in0=psf[pi][:, 0:512], in1=uT[:, g * 512:(g + 1) * 512], op=ALU.mult),
                            reads=["psf%d" % pi, "uT%d" % g], writes=["yaT"])
                    P.op("act", lambda e: e.activation(out=sqA[:], in_=yaT[:], func=AF.Square), reads=["yaT"], writes=["sqA"])
                    for g in range(6):
                        P.op("pe", lambda e, g=g: e.matmul(psf[5][:, 0:512], lhsT=onesb[:], rhs=sqA[:, g * 512:(g + 1) * 512],
                                                           start=(g == 0), stop=(g == 5)), reads=["sqA", "onesb"], writes=["psf5"])
                    rstd_rep("psf5", psf[5][:, 0:512], 768.0, rtmp[:], rrep[:], "rrepA")
                    for g in range(6):
                        P.op("dve", lambda e, g=g: e.scalar_tensor_tensor(
                            out=yan[:, g * 512:(g + 1) * 512], in0=yaT[:, g * 512:(g + 1) * 512], scalar=gA[:, g:g + 1],
                            in1=rrep[:], op0=ALU.mult, op1=ALU.mult), reads=["yaT", "gA", "rrepA"], writes=["yan"])
                    P.dma("sp", lambda e, gsl=gsl: e.dma_start(out=yT_d[0:6, :, gsl].rearrange("g p t -> p g t"),
                                                             in_=yan[:].rearrange("p (g t) -> p g t", g=6)),
                          reads=["yan"], writes=["yT_d"])
        P.barrier()
        M.reset()

    if upto >= 2:
        memb = M.alloc("memb", [128, 2 * D], BF16)
        memT = M.alloc("memT", [128, 16 * 256], BF16)
        wkv = [M.alloc("wkv%d" % i, [128, 16 * 512], BF16) for i in range(2)]
        for jx in range(2):
            P.dma("pool", lambda e, jx=jx: e.dma_start(out=memb[:, jx * D:(jx + 1) * D], in_=mem_d[jx * 128:(jx + 1) * 128, :]),
                  writes=["memb%d" % jx])
        for i in range(2):
            P.dma("pool", lambda e, i=i: e.dma_start(out=wkv[i][:], in_=wkv_d[i]), writes=["wkv%d" % i])
        for kc in range(16):
            hb = kc % 2
            for j in range(2):
                P.op("pe", lambda e, kc=kc, j=j, hb=hb: e.transpose(
                    out=psbs[hb][:, j * 128:(j + 1) * 128],
                    in_=memb[:, j * D + kc * 128: j * D + (kc + 1) * 128], identity=identb[:]),
                    reads=["memb%d" % j, "identb"], writes=["psb%d" % hb])
            copy_op(evac(kc), memT[:, kc * 256:(kc + 1) * 256], psbs[hb][:, 0:256], ["psb%d" % hb], ["memT"])
        for h in range(4):
            pi = h % 4
            for kc in range(16):
                P.op("pe", lambda e, pi=pi, kc=kc, h=h: e.matmul(
                    psf[pi][:, 0:256], lhsT=wkv[0][:, kc * 512 + h * 128: kc * 512 + (h + 1) * 128],
                    rhs=memT[:, kc * 256:(kc + 1) * 256], start=(kc == 0), stop=(kc == 15)),
                    reads=["wkv0", "memT"], writes=["psf%d" % pi])
            copy_op(evac(h), KmT[:, h * 256:(h + 1) * 256], psf[pi][:, 0:256], ["psf%d" % pi], ["KmT"])
        for blk in range(2):
            pi = 4 + blk
            for kc in range(16):
                P.op("pe", lambda e, pi=pi, kc=kc, blk=blk: e.matmul(
                    psf[pi][:, 0:512], lhsT=memT[:, kc * 256 + blk * 128: kc * 256 + (blk + 1) * 128],
                    rhs=wkv[1][:, kc * 512:(kc + 1) * 512], start=(kc == 0), stop=(kc == 15)),
                    reads=["wkv1", "memT"], writes=["psf%d" % pi])
            copy_op(evac(blk), Vm[:, blk * 512:(blk + 1) * 512], psf[pi][:, 0:512], ["psf%d" % pi], ["Vm"])
        P.barrier()
        M.reset()

        maskb = M.alloc("maskb", [128, 17 * 128], BF16)
        KT = M.alloc("KT", [128, 6 * S], BF16)
        Vt = M.alloc("Vt", [128, NT * 768], BF16)
        QT = [M.alloc("QT%d" % i, [128, 6 * 512], BF16) for i in range(2)]
        QmT = [M.alloc("QmT%d" % i, [128, 4 * 512], BF16) for i in range(2)]
        pTb = [M.alloc("pTb%d" % i, [128, 512], BF16) for i in range(3)]
        rd = [M.alloc("rd%d" % i, [128, 512], F32) for i in range(2)]
        ybT = M.alloc("ybT", [128, 6 * 512], BF16)
        ymT = M.alloc("ymT", [128, 4 * 512], BF16)
        sqB = M.alloc("sqB", [128, 6 * 512], BF16)
        ybn = M.alloc("ybn", [128, 6 * 512], BF16)
        rtmp3 = M.alloc("rtmp3", [128, 512], F32)
        rrep3 = M.alloc("rrep3", [128, 512], F32)

        P.dma("pool", lambda e: e.dma_start(out=maskb[:], in_=mask_d), writes=["maskb"])
        for h in range(6):
            P.dma("sp", lambda e, h=h: e.dma_start(out=KT[:, h * S:(h + 1) * S], in_=kT_d[h]), writes=["KT%d" % h])
        for q4 in range(4):
            P.dma("sp", lambda e, q4=q4: e.dma_start(
                out=Vt[:, q4 * 8 * 768:(q4 + 1) * 8 * 768].rearrange("p (b c) -> p b c", b=8),
                in_=v_d[q4 * 1024:(q4 + 1) * 1024, :].rearrange("(b p) c -> p b c", p=128)), writes=["Vt%d" % q4])

        psS = Ring([0, 1])
        psO = Ring([2, 3])
        psD = Ring([4, 5])
        pTR = Ring([0, 1, 2])
        rdR = Ring([0, 1])

        def load_q(G):
            b = G % 2
            gsl = slice(G * GT, (G + 1) * GT)
            P.dma("sp", lambda e: e.dma_start(out=QT[b][:].rearrange("p (h t) -> p h t", h=6),
                                              in_=qT_d[:, :, gsl].rearrange("h p t -> p h t")), writes=["QT%d" % b])
            P.dma("sp", lambda e: e.dma_start(out=QmT[b][:].rearrange("p (h t) -> p h t", h=4),
                                              in_=qmT_d[:, :, gsl].rearrange("h p t -> p h t")), writes=["QmT%d" % b])

        def rms_store(yT, ykey, nh, gains, gkey, nfeat, cbase, gsl):
            P.op("act", lambda e: e.activation(out=sqB[:, 0:nh * 512], in_=yT[:, 0:nh * 512], func=AF.Square),
                 reads=[ykey], writes=["sqB"])
            for g in range(nh):
                P.op("pe", lambda e, g=g: e.matmul(psf[0][:, 0:512], lhsT=onesb[:], rhs=sqB[:, g * 512:(g + 1) * 512],
                                                   start=(g == 0), stop=(g == nh - 1)), reads=["sqB", "onesb"], writes=["psf0"])
            rstd_rep("psf0", psf[0][:, 0:512], float(nfeat), rtmp3[:], rrep3[:], "rrepB")
            for g in range(nh):
                P.op("dve", lambda e, g=g: e.scalar_tensor_tensor(
                    out=ybn[:, g * 512:(g + 1) * 512], in0=yT[:, g * 512:(g + 1) * 512], scalar=gains[:, g:g + 1],
                    in1=rrep3[:], op0=ALU.mult, op1=ALU.mult), reads=[ykey, gkey, "rrepB"], writes=["ybn"])
            P.dma("sp", lambda e: e.dma_start(out=yT_d[cbase:cbase + nh, :, gsl].rearrange("g p t -> p g t"),
                                              in_=ybn[:, 0:nh * 512].rearrange("p (g t) -> p g t", g=nh)),
                  reads=["ybn"], writes=["yT_d"])

        load_q(0)
        tasks = []

        def flush_tasks():
            n = len(tasks)
            if n == 0:
                return
            tasks[0][0]()
            for i in range(n):
                if i + 1 < n:
                    tasks[i + 1][0]()
                tasks[i][1]()
            del tasks[:]

        for G in range(NG):
            b = G % 2
            gsl = slice(G * GT, (G + 1) * GT)
            if G + 1 < NG:
                load_q(G + 1)
            for hh in range(10):
                isB = hh < 6
                h = hh if isB else hh - 6
                po = psO.next()
                pd = psD.next()
                for qi in range(4):
                    n = 4 * G + qi
                    if isB:
                        kbs = list(range(max(0, n - 16), n + 1))
                        qap = QT[b][:, h * 512 + qi * 128: h * 512 + (qi + 1) * 128]
                        qkey = "QT%d" % b
                    else:
                        kbs = [0, 1]
                        qap = QmT[b][:, h * 512 + qi * 128: h * 512 + (qi + 1) * 128]
                        qkey = "QmT%d" % b
                    for c0 in range(0, len(kbs), 4):
                        ch = kbs[c0:c0 + 4]
                        L = len(ch)
                        ps = psS.next()
                        pt = pTR.next()
                        head_end = (qi == 3 and c0 + 4 >= len(kbs))

                        def stageA(ch=ch, L=L, ps=ps, pt=pt, isB=isB, h=h, qap=qap, qkey=qkey, n=n):
                            for jj, kb in enumerate(ch):
                                if isB:
                                    kap = KT[:, h * S + kb * 128: h * S + (kb + 1) * 128]
                                    kkey = "KT%d" % h
                                else:
                                    kap = KmT[:, h * 256 + kb * 128: h * 256 + (kb + 1) * 128]
                                    kkey = "KmT"
                                P.op("pe", lambda e, ps=ps, jj=jj, kap=kap, qap=qap: e.matmul(
                                    psf[ps][:, jj * 128:(jj + 1) * 128], lhsT=kap, rhs=qap, start=True, stop=True),
                                    reads=[kkey, qkey], writes=["psf%d" % ps])
                            P.op("act", lambda e, ps=ps, pt=pt, L=L: e.activation(
                                out=pTb[pt][:, 0:L * 128], in_=psf[ps][:, 0:L * 128], func=AF.Exp, scale=SCALE),
                                reads=["psf%d" % ps], writes=["pTb%d" % pt])
                            if isB:
                                i0 = ch[0] - (n - 16)
                                P.op("dve", lambda e, pt=pt, L=L, i0=i0: e.tensor_tensor(
                                    out=pTb[pt][:, 0:L * 128], in0=pTb[pt][:, 0:L * 128], in1=maskb[:, i0 * 128:(i0 + L) * 128], op=ALU.mult),
                                    reads=["pTb%d" % pt, "maskb"], writes=["pTb%d" % pt])

                        def stageB(ch=ch, c0=c0, nk=len(kbs), pt=pt, isB=isB, h=h, hh=hh, qi=qi, po=po, pd=pd, head_end=head_end, gsl=gsl):
                            for jj, kb in enumerate(ch):
                                first = (c0 + jj == 0)
                                last = (c0 + jj == nk - 1)
                                if isB:
                                    vap = Vt[:, kb * 768 + h * 128: kb * 768 + (h + 1) * 128]
                                    vkey = "Vt%d" % (kb // 8)
                                else:
                                    vap = Vm[:, kb * 512 + h * 128: kb * 512 + (h + 1) * 128]
                                    vkey = "Vm"
                                P.op("pe", lambda e, po=po, qi=qi, vap=vap, pt=pt, jj=jj, first=first, last=last: e.matmul(
                                    psf[po][:, qi * 128:(qi + 1) * 128], lhsT=vap, rhs=pTb[pt][:, jj * 128:(jj + 1) * 128],
                                    start=first, stop=last), reads=[vkey, "pTb%d" % pt], writes=["psf%d" % po])
                                P.op("pe", lambda e, pd=pd, qi=qi, pt=pt, jj=jj, first=first, last=last: e.matmul(
                                    psf[pd][:, qi * 128:(qi + 1) * 128], lhsT=onesb[:], rhs=pTb[pt][:, jj * 128:(jj + 1) * 128],
                                    start=first, stop=last), reads=["onesb", "pTb%d" % pt], writes=["psf%d" % pd])
                            if head_end:
                                ri = rdR.next()
                                P.op("dve", lambda e, pd=pd, ri=ri: e.reciprocal(out=rd[ri][:], in_=psf[pd][:, 0:512]),
                                     reads=["psf%d" % pd], writes=["rd%d" % ri])
                                ydst = ybT if isB else ymT
                                ykey = "ybT" if isB else "ymT"
                                P.op("dve", lambda e, po=po, ri=ri, ydst=ydst, h=h: e.tensor_tensor(
                                    out=ydst[:, h * 512:(h + 1) * 512], in0=psf[po][:, 0:512], in1=rd[ri][:], op=ALU.mult),
                                    reads=["psf%d" % po, "rd%d" % ri], writes=[ykey])
                                if hh == 5:
                                    rms_store(ybT, "ybT", 6, gB, "gB", 768, 6, gsl)
                                if hh == 9:
                                    rms_store(ymT, "ymT", 4, gM, "gM", 512, 12, gsl)
                        tasks.append((stageA, stageB))
            flush_tasks()
        P.barrier()
        M.reset()

    if upto >= 3:
        wout = M.alloc("wout", [128, 16 * D], BF16)
        lng = M.alloc("lng", [128, D], F32)
        lnb = M.alloc("lnb", [128, D], F32)
        wr = M.alloc("wr", [128, 16 * 32], F32)
        br = M.alloc("br", [1, 32], F32)
        tot = M.alloc("tot", [128, 32], F32)
        yTt = [M.alloc("yTt%d" % i, [128, 16 * 512], BF16) for i in range(2)]
        xt = [M.alloc("xt%d" % i, [128, D], F32) for i in range(2)]
        hb_ = xt
        x1t = [M.alloc("x1t%d" % i, [128, D], F32) for i in range(2)]
        x1b = [M.alloc("x1b%d" % i, [128, D], BF16) for i in range(2)]
        x1T = M.alloc("x1T", [128, 16 * 128], F32)
        stats = M.alloc("stats4", [128, 24], F32)
        mv = M.alloc("mv4", [128, 2], F32)
        tmp2 = M.alloc("tmp24", [128, 2], F32)
        lg = M.alloc("lg", [128, 32], F32)
        m8 = M.alloc("m8", [128, 8], F32)
        negm = M.alloc("negm", [128, 1], F32)
        ex = M.alloc("ex", [128, 32], F32)
        den = M.alloc("den", [128, 2], F32)
        selb = M.alloc("selb", [128, 32], BF16)

        for q4 in range(4):
            P.dma("pool", lambda e, q4=q4: e.dma_start(out=wout[:, q4 * 4 * D:(q4 + 1) * 4 * D],
                                                       in_=wout_d[:, q4 * 4 * D:(q4 + 1) * 4 * D]), writes=["wout"])
        P.dma("sp", lambda e, t=lng: e.dma_start(out=t[:], in_=ln1g_d.partition_broadcast(128)), writes=["lnconst_a"])
        P.dma("sp", lambda e, t=lnb: e.dma_start(out=t[:], in_=ln1b_d.partition_broadcast(128)), writes=["lnconst_b"])
        P.dma("sp", lambda e: e.dma_start(out=wr[:], in_=wr_d), writes=["wr"])
        P.dma("sp", lambda e: e.dma_start(out=br[:], in_=br_d), writes=["br"])
        P.op("dve", lambda e: e.memset(tot[:], 0.0), writes=["tot"])

        psX = Ring([0, 1, 2])
        psT = Ring([3, 4])

        def load_y(G):
            b = G % 2
            gsl = slice(G * GT, (G + 1) * GT)
            P.dma("sp", lambda e: e.dma_start(out=yTt[b][:].rearrange("p (c t) -> p c t", c=16),
                                              in_=yT_d[:, :, gsl].rearrange("c p t -> p c t")), writes=["yTt%d" % b])

        def load_x(T):
            b = T % 2
            P.dma("sp", lambda e: e.dma_start(out=xt[b][:], in_=x_d[T * 128:(T + 1) * 128, :]),
                  writes=["xt%d" % b] + ["hb%d_%d" % (b, cc) for cc in range(4)])

        load_y(0)
        load_x(0)
        for T in range(NT):
            G, j = T // 4, T % 4
            b = T % 2
            yb = G % 2
            if j == 0 and G + 1 < NG:
                load_y(G + 1)
            if T + 1 < NT:
                load_x(T + 1)
            hk = "hb%d" % b
            for cc in range(4):
                pi = psX.next()
                for fc in range(16):
                    P.op("pe", lambda e, pi=pi, fc=fc, cc=cc, j=j, yb=yb: e.matmul(
                        psf[pi][:, 0:512], lhsT=yTt[yb][:, fc * 512 + j * 128: fc * 512 + (j + 1) * 128],
                        rhs=wout[:, fc * D + cc * 512: fc * D + (cc + 1) * 512], start=(fc == 0), stop=(fc == 15)),
                        reads=["yTt%d" % yb, "wout"], writes=["psf%d" % pi])
                P.op("dve", lambda e, pi=pi, cc=cc, b=b: e.scalar_tensor_tensor(
                    out=hb_[b][:, cc * 512:(cc + 1) * 512], in0=xt[b][:, cc * 512:(cc + 1) * 512], scalar=DN_ALPHA,
                    in1=psf[pi][:, 0:512], op0=ALU.mult, op1=ALU.add), reads=["xt%d" % b, "psf%d" % pi], writes=[hk + "_%d" % cc])
            layer_norm_rows(hb_[b][:], [hk + "_%d" % cc for cc in range(4)], lng[:], lnb[:], x1t[b][:], "x1t%d" % b, stats, mv, tmp2, D, "l1_")
            P.dma("sp", lambda e, T=T, b=b: e.dma_start(out=x1_d[T * 128:(T + 1) * 128, :], in_=x1t[b][:]),
                  reads=["x1t%d" % b], writes=["x1_d"])
            P.op("act", lambda e, b=b: e.activation(out=x1b[b][:], in_=x1t[b][:], func=AF.Copy),
                 reads=["x1t%d" % b], writes=["x1b%d" % b])
            P.dma("sp", lambda e, T=T, b=b: e.dma_start(out=x1b_d[T * 128:(T + 1) * 128, :], in_=x1b[b][:]),
                  reads=["x1b%d" % b], writes=["x1b_d"])
            for k4 in range(4):
                pi = psT.next()
                for kk in range(4):
                    kc = k4 * 4 + kk
                    P.op("pe", lambda e, pi=pi, kk=kk, kc=kc, b=b: e.transpose(
                        out=psf[pi][:, kk * 128:(kk + 1) * 128], in_=x1t[b][:, kc * 128:(kc + 1) * 128], identity=identf),
                        reads=["x1t%d" % b, "cst"], writes=["psf%d" % pi])
                copy_op("act", x1T[:, k4 * 512:(k4 + 1) * 512], psf[pi][:, 0:512], ["psf%d" % pi], ["x1T"])
            for kc in range(16):
                P.op("pe", lambda e, kc=kc: e.matmul(psf[5][:, 0:32], lhsT=x1T[:, kc * 128:(kc + 1) * 128],
                                                     rhs=wr[:, kc * 32:(kc + 1) * 32], start=(kc == 0), stop=False),
                     reads=["x1T", "wr"], writes=["psf5"])
            P.op("pe", lambda e: e.matmul(psf[5][:, 0:32], lhsT=onesrow32[0:1, 0:128], rhs=br[0:1, :], start=False, stop=True),
                 reads=["onesrow32", "br"], writes=["psf5"])
            tsl = slice(T * 32, (T + 1) * 32)
            P.op("dve", lambda e: e.tensor_copy(out=lg[:], in_=psf[5][:, 0:32]), reads=["psf5"], writes=["lg"])
            P.op("dve", lambda e: e.max(out=m8[:], in_=lg[:]), reads=["lg"], writes=["m8"])
            P.op("dve", lambda e, tsl=tsl: e.tensor_single_scalar(out=sel_all[:, tsl], in_=lg[:], scalar=m8[:, 3:4], op=ALU.is_ge),
                 reads=["lg", "m8"], writes=["sel"])
            P.op("dve", lambda e: e.tensor_single_scalar(out=negm[:], in_=m8[:, 0:1], scalar=-1.0, op=ALU.mult), reads=["m8"], writes=["negm"])
            P.op("act", lambda e: e.activation(out=ex[:], in_=lg[:], func=AF.Exp, bias=negm[:, 0:1], scale=1.0),
                 reads=["lg", "negm"], writes=["ex"])
            P.op("dve", lambda e, tsl=tsl: e.tensor_tensor(out=ex[:], in0=ex[:], in1=sel_all[:, tsl], op=ALU.mult),
                 reads=["ex", "sel"], writes=["ex"])
            P.op("dve", lambda e: e.reduce_sum(out=den[:, 0:1], in_=ex[:], axis=AX.X), reads=["ex"], writes=["den"])
            P.op("dve", lambda e: e.reciprocal(out=den[:, 1:2], in_=den[:, 0:1]), reads=["den"], writes=["rden"])
            P.op("dve", lambda e, tsl=tsl: e.tensor_single_scalar(out=gate_all[:, tsl], in_=ex[:], scalar=den[:, 1:2], op=ALU.mult),
                 reads=["ex", "rden"], writes=["gate"])
            P.op("dve", lambda e, tsl=tsl: e.tensor_copy(out=selb[:], in_=sel_all[:, tsl]), reads=["sel"], writes=["selb"])
            P.op("pe", lambda e: e.matmul(psf[5][:, 64:96], lhsT=trib[:], rhs=selb[:], start=True, stop=True),
                 reads=["trib", "selb", "lg"], writes=["psf5"])
            P.op("pe", lambda e: e.matmul(psf[5][:, 96:128], lhsT=onesb[:], rhs=selb[:], start=True, stop=True),
                 reads=["onesb", "selb"], writes=["psf5"])
            P.op("dve", lambda e, tsl=tsl: e.tensor_tensor(out=rank_all[:, tsl], in0=psf[5][:, 64:96], in1=tot[:], op=ALU.add),
                 reads=["psf5", "tot"], writes=["rank"])
            P.op("dve", lambda e: e.tensor_tensor(out=tot[:], in0=psf[5][:, 96:128], in1=tot[:], op=ALU.add),
                 reads=["psf5", "tot"], writes=["tot", "psf5"])

        ci = M.alloc("ci", [128, 32], I32)
        padded = M.alloc("padded", [128, 32], F32)
        csA = M.alloc("csA", [128, 32], F32)
        csB = M.alloc("csB", [128, 32], F32)
        pstart = M.alloc("pstart", [128, 32], F32)
        cmpt = M.alloc("cmpt", [128, 32], F32)
        bex = M.alloc("bex", [128, 1], F32)
        bexi = M.alloc("bexi", [128, 1], I32)
        dst = M.alloc("dst", [128, 32], F32)
        m8d = M.alloc("m8d", [128, 8], F32)
        key2 = M.alloc("key2", [128, 32], F32)
        m8e = M.alloc("m8e", [128, 8], F32)
        tq = M.alloc("tq", [128, 32], F32)
        xsc = [M.alloc("xsc%d" % i, [128, D], BF16) for i in range(3)]

        P.op("dve", lambda e: e.tensor_copy(out=ci[:], in_=tot[:]), reads=["tot"], writes=["ci"])
        P.op("dve", lambda e: e.tensor_single_scalar(out=ci[:], in_=ci[:], scalar=511, op=ALU.add), reads=["ci"], writes=["ci"])
        P.op("dve", lambda e: e.tensor_scalar(out=ci[:], in0=ci[:], scalar1=9, scalar2=9, op0=ALU.arith_shift_right,
                                              op1=ALU.logical_shift_left), reads=["ci"], writes=["ci"])
        P.op("dve", lambda e: e.tensor_copy(out=padded[:], in_=ci[:]), reads=["ci"], writes=["padded"])
        P.op("dve", lambda e: e.tensor_copy(out=csA[:], in_=padded[:]), reads=["padded"], writes=["csA"])
        cur, oth, ck, ok_ = csA, csB, "csA", "csB"
        for s in (1, 2, 4, 8, 16):
            P.op("dve", lambda e, cur=cur, oth=oth: e.tensor_copy(out=oth[:], in_=cur[:]), reads=[ck], writes=[ok_])
            P.op("dve", lambda e, cur=cur, oth=oth, s=s: e.tensor_tensor(out=oth[:, s:32], in0=cur[:, s:32], in1=cur[:, 0:32 - s], op=ALU.add),
                 reads=[ck, ok_], writes=[ok_])
            cur, oth, ck, ok_ = oth, cur, ok_, ck
        pend_t, pend_k = cur, ck
        P.op("dve", lambda e: e.tensor_tensor(out=pstart[:], in0=pend_t[:], in1=padded[:], op=ALU.subtract),
             reads=[pend_k, "padded"], writes=["pstart"])
        Erep = M.alloc("Erep", [128, 64], F32)
        pc = M.alloc("pc", [128, 8], F32)
        idxf = M.alloc("idxf", [128, 8 * 64], F32)
        for bq in range(NBLK):
            P.op("dve", lambda e, bq=bq: e.tensor_single_scalar(out=cmpt[:], in_=pend_t[:], scalar=512.0 * bq, op=ALU.is_le),
                 reads=[pend_k], writes=["cmpt"])
            P.op("dve", lambda e, bq=bq: e.reduce_sum(out=Erep[:, bq:bq + 1], in_=cmpt[:], axis=AX.X), reads=["cmpt"], writes=["Erep"])
        P.op("dve", lambda e: e.tensor_single_scalar(out=Erep[:], in_=Erep[:], scalar=31.0, op=ALU.min), reads=["Erep"], writes=["Erep"])
        for l in range(8):
            P.op("dve", lambda e, l=l: e.tensor_scalar(out=pc[:, l:l + 1], in0=thr, scalar1=1.0 / 512.0, scalar2=128.0 * l,
                                                       op0=ALU.mult, op1=ALU.add), reads=["cst"], writes=["pc"])
        for l in range(8):
            P.op("dve", lambda e, l=l: e.tensor_scalar(out=idxf[:, l * 64:(l + 1) * 64], in0=Erep[:], scalar1=1024.0, scalar2=pc[:, l:l + 1],
                                                       op0=ALU.mult, op1=ALU.add), reads=["Erep", "pc"], writes=["idxf"])
        P.op("dve", lambda e: e.tensor_copy(out=idx_gu[:], in_=idxf[:]), reads=["idxf"], writes=["idx_gu"])
        for l in range(4):
            P.op("dve", lambda e, l=l: e.tensor_scalar(out=idxf[:, l * 64:(l + 1) * 64], in0=Erep[:], scalar1=512.0, scalar2=pc[:, l:l + 1],
                                                       op0=ALU.mult, op1=ALU.add), reads=["Erep", "pc", "idx_gu"], writes=["idxf"])
        P.op("dve", lambda e: e.tensor_copy(out=idx_d[:], in_=idxf[:, 0:256]), reads=["idxf"], writes=["idx_d"])
        P.op("dve", lambda e: e.tensor_scalar(out=idxf[:, 0:64], in0=Erep[:], scalar1=128.0, scalar2=pc[:, 0:1],
                                              op0=ALU.mult, op1=ALU.add), reads=["Erep", "pc", "idx_d"], writes=["idxf"])
        P.op("dve", lambda e: e.tensor_copy(out=idx_b[:], in_=idxf[:, 0:64]), reads=["idxf"], writes=["idx_b"])
        P.op("dve", lambda e: e.tensor_copy(out=idx_e[:], in_=Erep[:]), reads=["Erep"], writes=["idx_e"])
        P.dma("sp", lambda e: e.dma_start(out=bexp_d[0:64, :].rearrange("b o -> o b"), in_=idx_e[0:1, :]), reads=["idx_e"], writes=["bexp_d"])
        xscR = Ring([0, 1, 2])
        for T in range(NT):
            tsl = slice(T * 32, (T + 1) * 32)
            t4 = slice(T * 4, (T + 1) * 4)
            P.op("dve", lambda e, tsl=tsl: e.tensor_tensor(out=dst[:], in0=rank_all[:, tsl], in1=pstart[:], op=ALU.add),
                 reads=["rank", "pstart"], writes=["dst"])
            P.op("dve", lambda e, tsl=tsl: e.scalar_tensor_tensor(out=dst[:], in0=dst[:], scalar=1.0, in1=sel_all[:, tsl],
                                                                  op0=ALU.add, op1=ALU.mult), reads=["dst", "sel"], writes=["dst"])
            P.op("dve", lambda e: e.tensor_single_scalar(out=dst[:], in_=dst[:], scalar=-1.0, op=ALU.add), reads=["dst"], writes=["dst"])
            P.op("dve", lambda e: e.max(out=m8d[:], in_=dst[:]), reads=["dst"], writes=["m8d"])
            P.op("dve", lambda e, t4=t4: e.tensor_copy(out=dest4i[:, t4], in_=m8d[:, 0:4]), reads=["m8d"], writes=["dest4i"])
            P.op("dve", lambda e, tsl=tsl: e.tensor_tensor(out=key2[:], in0=sel_all[:, tsl], in1=iota1, op=ALU.mult),
                 reads=["sel", "cst"], writes=["key2"])
            P.op("dve", lambda e: e.max(out=m8e[:], in_=key2[:]), reads=["key2"], writes=["m8e"])
            for k in range(4):
                P.op("dve", lambda e, k=k, tsl=tsl: e.scalar_tensor_tensor(out=tq[:], in0=key2[:], scalar=m8e[:, k:k + 1],
                                                                           in1=gate_all[:, tsl], op0=ALU.is_equal, op1=ALU.mult),
                     reads=["key2", "m8e", "gate"], writes=["tq"])
                P.op("dve", lambda e, k=k, T=T: e.reduce_sum(out=gate4[:, T * 4 + k: T * 4 + k + 1], in_=tq[:], axis=AX.X),
                     reads=["tq"], writes=["gate4"])
            xi = xscR.next()
            P.dma("sp", lambda e, T=T, xi=xi: e.dma_start(out=xsc[xi][:], in_=x1b_d[T * 128:(T + 1) * 128, :]),
                  reads=["x1b_d"], writes=["xsc%d" % xi])
            for k in range(4):
                P.dma("pool", lambda e, T=T, k=k, xi=xi: e.indirect_dma_start(
                    out=xs_d, out_offset=bass.IndirectOffsetOnAxis(ap=dest4i[:, T * 4 + k: T * 4 + k + 1], axis=0),
                    in_=xsc[xi][:], in_offset=None, bounds_check=get_bc(e), oob_is_err=False),
                    reads=["xsc%d" % xi, "dest4i"], writes=["xs_d"])
        if "rt_s" in dbg:
            P.dma("sp", lambda e: e.dma_start(out=rt_d[:, 0:NT * 4], in_=gate4[:]), reads=["gate4"], writes=["rt_d"])
            P.dma("sp", lambda e: e.dma_start(out=rt_d[:, NT * 4:NT * 8].bitcast(I32), in_=dest4i[:]), reads=["dest4i"], writes=["rt_d"])
        P.barrier()
        M.reset()

    if upto >= 4:
        wsl = [M.alloc("wsl%d" % i, [128, 16 * 512], BF16) for i in range(4)]
        xsb = [M.alloc("xsb%d" % i, [128, 4 * D], BF16) for i in range(2)]
        xbT2 = [M.alloc("xbT%d" % i, [128, 16 * 512], BF16) for i in range(2)]
        hT = M.alloc("hT", [128, 16 * 512], BF16)
        gc = [M.alloc("gc%d" % i, [128, 512], F32) for i in range(2)]
        sg = [M.alloc("sg%d" % i, [128, 512], F32) for i in range(2)]
        uc = [M.alloc("uc%d" % i, [128, 512], F32) for i in range(2)]
        yst = [M.alloc("yst%d" % i, [128, 512], F32) for i in range(4)]
        bgu = [M.alloc("bgu%d" % i, [128, 32], F32) for i in range(2)]
        bdr = [M.alloc("bdr%d" % i, [2, D], BF16) for i in range(2)]

        psG = Ring([0, 1])
        psU = Ring([2, 3])
        psY = Ring([4, 5])
        ystR = Ring([0, 1, 2, 3])
        tR = Ring([0, 1])
        wl = [(blk, l) for blk in range(NBLK) for l in range(12)]
        nw = [0]

        def issue_w(upto_idx):
            while nw[0] <= upto_idx and nw[0] < len(wl):
                blk, l = wl[nw[0]]
                i = nw[0] % 4
                if l == 0:
                    bb = blk % 2
                    P.dma("pool", lambda e, blk=blk, bb=bb: e.indirect_dma_start(
                        out=bgu[bb][:, :], out_offset=None, in_=bgu_d,
                        in_offset=bass.IndirectOffsetOnAxis(ap=idx_b[:, blk:blk + 1], axis=0),
                        bounds_check=get_bc(e), oob_is_err=False), reads=["idx"], writes=["bgu%d" % bb])
                    P.dma("pool", lambda e, blk=blk, bb=bb: e.indirect_dma_start(
                        out=bdr[bb][0:2, :], out_offset=None, in_=bd_d,
                        in_offset=bass.IndirectOffsetOnAxis(ap=idx_e[0:2, blk:blk + 1], axis=0),
                        bounds_check=get_bc(e), oob_is_err=False), reads=["idx"], writes=["bdr%d" % bb])

                def ld_w(e, blk=blk, l=l, i=i):
                    if l < 8:
                        src, ia = wgu_d, idx_gu[:, l * 64 + blk: l * 64 + blk + 1]
                    else:
                        src, ia = wd_d, idx_d[:, (l - 8) * 64 + blk: (l - 8) * 64 + blk + 1]
                    return e.indirect_dma_start(out=wsl[i][:, :], out_offset=None, in_=src,
                                                in_offset=bass.IndirectOffsetOnAxis(ap=ia, axis=0),
                                                bounds_check=get_bc(e), oob_is_err=False)
                P.dma("pool", ld_w, reads=["idx"], writes=["wsl%d" % i])
                nw[0] += 1

        def load_xs(blk):
            bb = blk % 2
            P.dma("sp", lambda e: e.dma_start(out=xsb[bb][:].rearrange("p (j d) -> p j d", j=4),
                                              in_=xs_d[blk * 512:(blk + 1) * 512, :].rearrange("(j p) d -> p j d", p=128)),
                  reads=["xs_d"], writes=["xsb%d" % bb])

        def emit_transposes(blk):
            bb = blk % 2
            xb_t = xbT2[bb]
            for kc in range(16):
                hb = kc % 2
                for j in range(4):
                    P.op("pe", lambda e, kc=kc, j=j, hb=hb, bb=bb: e.transpose(
                        out=psbs[hb][:, j * 128:(j + 1) * 128],
                        in_=xsb[bb][:, j * D + kc * 128: j * D + (kc + 1) * 128], identity=identb[:]),
                        reads=["xsb%d" % bb, "identb"], writes=["psb%d" % hb])
                copy_op(evac(kc), xb_t[:, kc * 512:(kc + 1) * 512], psbs[hb][:, 0:512], ["psb%d" % hb], ["xbT%d" % bb])

        load_xs(0)
        load_xs(1)
        ecount = [0]
        issue_w(2)
        emit_transposes(0)
        for blk in range(NBLK):
            bb = blk % 2
            xbT = xbT2[bb]
            xkey = "xbT%d" % bb
            issue_w(blk * 12 + 2)
            for l in range(12):
                wi_idx = blk * 12 + l
                issue_w(wi_idx + 3)
                wi = wi_idx % 4
                wt = wsl[wi]
                wkey = "wsl%d" % wi
                if l < 8:
                    for s2 in range(2):
                        fci = 2 * l + s2
                        pg = psG.next()
                        pu = psU.next()
                        for kc in range(16):
                            P.op("pe", lambda e, pg=pg, kc=kc, s2=s2, wt=wt, xbT=xbT: e.matmul(
                                psf[pg][:, 0:512], lhsT=wt[:, kc * 512 + s2 * 128: kc * 512 + (s2 + 1) * 128],
                                rhs=xbT[:, kc * 512:(kc + 1) * 512], start=(kc == 0), stop=(kc == 15)),
                                reads=[wkey, xkey], writes=["psf%d" % pg])
                        for kc in range(16):
                            P.op("pe", lambda e, pu=pu, kc=kc, s2=s2, wt=wt, xbT=xbT: e.matmul(
                                psf[pu][:, 0:512], lhsT=wt[:, kc * 512 + 256 + s2 * 128: kc * 512 + 256 + (s2 + 1) * 128],
                                rhs=xbT[:, kc * 512:(kc + 1) * 512], start=(kc == 0), stop=(kc == 15)),
                                reads=[wkey, xkey], writes=["psf%d" % pu])
                        ti = tR.next()
                        P.op("dve", lambda e, pg=pg, ti=ti, fci=fci, bb=bb: e.tensor_scalar(
                            out=gc[ti][:], in0=psf[pg][:, 0:512], scalar1=bgu[bb][:, fci:fci + 1], scalar2=7.0,
                            op0=ALU.add, op1=ALU.min), reads=["psf%d" % pg, "bgu%d" % bb], writes=["gc%d" % ti])
                        P.op("act", lambda e, ti=ti: e.activation(out=sg[ti][:], in_=gc[ti][:], func=AF.Sigmoid, scale=1.702),
                             reads=["gc%d" % ti], writes=["sg%d" % ti])
                        P.op("dve", lambda e, pu=pu, ti=ti, fci=fci, bb=bb: e.tensor_scalar(
                            out=uc[ti][:], in0=psf[pu][:, 0:512], scalar1=bgu[bb][:, 16 + fci:17 + fci], scalar2=7.0,
                            op0=ALU.add, op1=ALU.min), reads=["psf%d" % pu, "bgu%d" % bb], writes=["uc%d" % ti])
                        P.op("dve", lambda e, ti=ti: e.tensor_scalar(
                            out=uc[ti][:], in0=uc[ti][:], scalar1=-7.0, scalar2=1.0, op0=ALU.max, op1=ALU.add),
                            reads=["uc%d" % ti], writes=["uc%d" % ti])
                        P.op("dve", lambda e, ti=ti: e.tensor_tensor(out=uc[ti][:], in0=uc[ti][:], in1=gc[ti][:], op=ALU.mult),
                             reads=["uc%d" % ti, "gc%d" % ti], writes=["uc%d" % ti])
                        P.op("dve", lambda e, ti=ti, fci=fci: e.tensor_tensor(
                            out=hT[:, fci * 512:(fci + 1) * 512], in0=uc[ti][:], in1=sg[ti][:], op=ALU.mult),
                            reads=["uc%d" % ti, "sg%d" % ti], writes=["hT"])
                else:
                    if l == 8 and blk + 1 < NBLK:
                        emit_transposes(blk + 1)
                        if blk + 2 < NBLK:
                            load_xs(blk + 2)
                    lc = l - 8
                    for j in range(4):
                        py = psY.next()
                        for fc in range(16):
                            P.op("pe", lambda e, py=py, fc=fc, j=j, wt=wt: e.matmul(
                                psf[py][:, 0:512], lhsT=hT[:, fc * 512 + j * 128: fc * 512 + (j + 1) * 128],
                                rhs=wt[:, fc * 512:(fc + 1) * 512], start=(fc == 0), stop=False),
                                reads=[wkey, "hT"], writes=["psf%d" % py])
                        P.op("pe", lambda e, py=py, lc=lc, bb=bb: e.matmul(
                            psf[py][:, 0:512], lhsT=onesrow[0:1, 0:128], rhs=bdr[bb][0:1, lc * 512:(lc + 1) * 512],
                            start=False, stop=True), reads=["onesrow", "bdr%d" % bb], writes=["psf%d" % py])
                        yi = ystR.next()
                        ecount[0] += 1
                        copy_op(evac(ecount[0]), yst[yi][:], psf[py][:, 0:512], ["psf%d" % py], ["yst%d" % yi])
                        P.dma("sp", lambda e, blk=blk, j=j, lc=lc, yi=yi: e.dma_start(
                            out=ys_d[blk * 512 + j * 128: blk * 512 + (j + 1) * 128, lc * 512:(lc + 1) * 512], in_=yst[yi][:]),
                            reads=["yst%d" % yi], writes=["ys_d"])
        P.barrier()
        M.reset()

    if upto >= 5:
        lng = M.alloc("lng6", [128, D], F32)
        lnb = M.alloc("lnb6", [128, D], F32)
        yg = [M.alloc("yg%d" % i, [128, 4 * D], F32) for i in range(2)]
        x1l = [M.alloc("x1l%d" % i, [128, D], F32) for i in range(2)]
        acc = [M.alloc("acc%d" % i, [128, D], F32) for i in range(2)]
        ot = [M.alloc("ot%d" % i, [128, D], F32) for i in range(2)]
        stats = M.alloc("stats6", [128, 24], F32)
        mv = M.alloc("mv6", [128, 2], F32)
        tmp2 = M.alloc("tmp26", [128, 2], F32)
        P.dma("sp", lambda e, t=lng: e.dma_start(out=t[:], in_=ln2g_d.partition_broadcast(128)), writes=["lnconst_a"])
        P.dma("sp", lambda e, t=lnb: e.dma_start(out=t[:], in_=ln2b_d.partition_broadcast(128)), writes=["lnconst_b"])

        def gather(T):
            b = T % 2
            for k in range(4):
                P.dma("pool", lambda e, k=k: e.indirect_dma_start(
                    out=yg[b][:, k * D:(k + 1) * D], out_offset=None, in_=ys_d,
                    in_offset=bass.IndirectOffsetOnAxis(ap=dest4i[:, T * 4 + k: T * 4 + k + 1], axis=0),
                    bounds_check=get_bc(e), oob_is_err=False), reads=["ys_d", "dest4i"], writes=["yg%d_%d" % (b, k)])
            P.dma("sp", lambda e: e.dma_start(out=x1l[b][:], in_=x1_d[T * 128:(T + 1) * 128, :]), reads=["x1_d"], writes=["x1l%d" % b])

        gather(0)
        for T in range(NT):
            b = T % 2
            if T + 1 < NT:
                gather(T + 1)
            ak = "acc%d" % b
            P.op("act", lambda e, b=b: e.mul(acc[b][:], x1l[b][:], DN_ALPHA),
                 reads=["x1l%d" % b], writes=[ak])
            for k in range(4):
                P.op("dve", lambda e, b=b, k=k, T=T: e.scalar_tensor_tensor(
                    out=acc[b][:], in0=yg[b][:, k * D:(k + 1) * D], scalar=gate4[:, T * 4 + k: T * 4 + k + 1], in1=acc[b][:],
                    op0=ALU.mult, op1=ALU.add), reads=["yg%d_%d" % (b, k), "gate4", ak], writes=[ak])
            layer_norm_rows(acc[b][:], [ak], lng[:], lnb[:], ot[b][:], "ot%d" % b, stats, mv, tmp2, D, "l2_")
            P.dma("sp", lambda e, T=T, b=b: e.dma_start(out=out_d[T * 128:(T + 1) * 128, :], in_=ot[b][:]),
                  reads=["ot%d" % b], writes=["out_d"])
    P.emit()
    return nc


def _mask_const():
    kj = np.arange(128)[:, None, None]
    i = np.arange(17)[None, :, None]
    qi = np.arange(128)[None, None, :]
    o = 16 - i
    dl = 128 * o + qi - kj
    m = ((dl >= 0) & (dl <= 128)).astype(np.float32)
    m += ((dl >= 0) & (dl <= 512) & (dl % 4 == 0)).astype(np.float32)
    m += ((dl >= 0) & (dl <= 2048) & (dl % 16 == 0)).astype(np.float32)
    return np.ascontiguousarray(m.reshape(128, 17 * 128))


def _consts():
    c = np.zeros((128, 417), np.float32)
    c[:, 0:128] = np.eye(128, dtype=np.float32)
    tp = np.arange(128)[:, None]
    t = np.arange(128)[None, :]
    c[:, 128:256] = (tp < t).astype(np.float32)
    c[:, 256:288] = np.arange(1, 33, dtype=np.float32)[None, :]
    c[:, 288] = np.arange(128, dtype=np.float32) * 512.0
    c[:, 289:417] = 1.0
    return c


def prep_shared(inp):
    f = lambda a: np.ascontiguousarray(np.asarray(a, dtype=np.float32))
    w_in = f(inp["w_in"])[0]
    w_in_p = np.zeros((D, 4608), np.float32)
    w_in_p[:, :4352] = w_in
    sh = {}
    sh["w_in_r"] = np.ascontiguousarray(w_in_p.reshape(16, 128, 9, 512).transpose(2, 1, 0, 3)).reshape(9, 128, 16 * 512)
    sh["w_spatial"] = f(inp["w_spatial"])[0]
    sh["b_spatial"] = f(inp["b_spatial"])[0].reshape(1, 768)
    sh["a_ln_g"] = f(inp["a_ln_g"]).reshape(1, 768)
    sh["a_ln_b"] = f(inp["a_ln_b"]).reshape(1, 768)
    sh["w_kv_r"] = np.ascontiguousarray(f(inp["w_mem_kv"])[0].reshape(16, 128, 2, 512).transpose(2, 1, 0, 3)).reshape(2, 128, 16 * 512)
    sh["norm_a_g"] = f(inp["norm_a_g"]).reshape(768)
    sh["norm_b_g"] = f(inp["norm_b_g"]).reshape(768)
    sh["norm_m_g"] = f(inp["norm_m_g"]).reshape(512)
    sh["w_out_r"] = np.ascontiguousarray(f(inp["w_out"])[0].reshape(16, 128, D).transpose(1, 0, 2)).reshape(128, 16 * D)
    sh["ln1_g"] = f(inp["ln1_g"]).reshape(1, D)
    sh["ln1_b"] = f(inp["ln1_b"]).reshape(1, D)
    sh["w_router_r"] = np.ascontiguousarray(f(inp["w_router"])[0].reshape(16, 128, 32).transpose(1, 0, 2)).reshape(128, 16 * 32)
    sh["b_router"] = f(inp["b_router"]).reshape(1, 32)
    wgu = f(inp["w_gate_up"])[0]
    NEP = wgu.shape[0]
    v = wgu.reshape(NEP, 16, 128, 2, 8, 256)
    sh["w_gu_r"] = np.ascontiguousarray(v.transpose(0, 4, 2, 1, 3, 5)).reshape(NEP * 8 * 128, 16 * 512)
    sh["b_gu_r"] = np.ascontiguousarray(f(inp["b_gate_up"])[0].reshape(NE, 32, 128).transpose(0, 2, 1)).reshape(NE * 128, 32)
    wd = f(inp["w_down"])[0]
    v = wd.reshape(NEP, 16, 128, 4, 512)
    sh["w_d_r"] = np.ascontiguousarray(v.transpose(0, 3, 2, 1, 4)).reshape(NEP * 4 * 128, 16 * 512)
    sh["b_down"] = f(inp["b_down"])[0].reshape(NE, D)
    sh["ln2_g"] = f(inp["ln2_g"]).reshape(1, D)
    sh["ln2_b"] = f(inp["ln2_b"]).reshape(1, D)
    sh["cst"] = _consts()
    sh["maskc"] = _mask_const()
    return sh


def kernel(**inputs):
    x = np.asarray(inputs["x"], dtype=np.float32)
    mem = np.asarray(inputs["mem"], dtype=np.float32)
    sh = prep_shared(inputs)
    nc = build()
    in_maps = []
    for c in range(8):
        m = dict(sh)
        m["x"] = np.ascontiguousarray(x[c])
        m["mem"] = np.ascontiguousarray(mem[c])
        in_maps.append(m)
    res = run_bass_kernel_spmd(nc, in_maps, core_ids=list(range(8)))
    return np.stack([np.asarray(r["out"], dtype=np.float32) for r in res.results], axis=0)
```
